# Optimizing a Trainium2 kernel written in Bass

```python
import math
import jax
import jax.numpy as jnp
from jax import lax
import numpy as np

D_MODEL = 1024
BATCH = 2
SEQ = 16384
DEPTH = 2

GRID_W = 64
CTX_LEN = 256
D_CONV = 512
CONV_K = 31
D_SSD = 512
SSD_HEADDIM = 64
SSD_HEADS = D_SSD // SSD_HEADDIM
SSD_GROUPS = 2
SSD_STATE = 128
SSD_CONV_K = 5
SSD_CHUNK = 128
D_BC = SSD_GROUPS * SSD_STATE
D_XBC = D_SSD + 2 * D_BC
D_MIX = D_CONV + D_SSD
D_IN = 2 * D_CONV + D_SSD + D_XBC + 2 * SSD_HEADS
MOE_GROUPS = 4
MOE_PER_GROUP = 4
N_EXPERTS = MOE_GROUPS * MOE_PER_GROUP
MOE_TOPK = 2
EXPERT_FF = 256
RMS_EPS = 1e-6
LN_EPS = 1e-5

kernel_name = "hymba_conformer_ssd_hmoe_prefix_dit"


def rms_norm(x, g):
    xf = x.astype(jnp.float32)
    y = xf * lax.rsqrt(jnp.mean(jnp.square(xf), axis=-1, keepdims=True) + RMS_EPS)
    return (y * g.astype(jnp.float32)).astype(x.dtype)


def layer_norm(x, g, b):
    xf = x.astype(jnp.float32)
    mu = jnp.mean(xf, axis=-1, keepdims=True)
    var = jnp.mean(jnp.square(xf - mu), axis=-1, keepdims=True)
    y = (xf - mu) * lax.rsqrt(var + LN_EPS)
    return (y * g.astype(jnp.float32) + b.astype(jnp.float32)).astype(x.dtype)


def modulate(h, shift, scale):
    return h * (1.0 + scale) + shift


def ada_mod(cvec, w_ada, b_ada, n):
    m = jax.nn.silu(cvec) @ w_ada[:, :n * D_MODEL] + b_ada[:n * D_MODEL]
    return jnp.split(m, n, axis=-1)


def dwconv_seq(v, w):
    k, ch = w.shape
    return lax.conv_general_dilated(
        v, w[:, None, :].astype(v.dtype), (1,), [(k // 2, k // 2)],
        dimension_numbers=("NWC", "WIO", "NWC"), feature_group_count=ch)


def dwconv_grid(v, w, axis):
    k, ch = w.shape
    if axis == 1:
        taps, pad = w[:, None, None, :], [(k // 2, k // 2), (0, 0)]
    else:
        taps, pad = w[None, :, None, :], [(0, 0), (k // 2, k // 2)]
    return lax.conv_general_dilated(
        v, taps.astype(v.dtype), (1, 1), pad,
        dimension_numbers=("NHWC", "HWIO", "NHWC"), feature_group_count=ch)


def split_proj(proj):
    o1, o2, o3, o4 = D_CONV, 2 * D_CONV, 2 * D_CONV + D_SSD, 2 * D_CONV + D_SSD + D_XBC
    return proj[..., :o1], proj[..., o1:o2], proj[..., o2:o3], proj[..., o3:o4], proj[..., o4:]


def conformer_conv(u, gate, p, rows):
    v = u * jax.nn.sigmoid(gate)
    if rows is None:
        y = dwconv_seq(v, p["conv_w"])
    else:
        b, l, ch = v.shape
        half = ch // 2
        vg = v.reshape(b, rows, GRID_W, ch)
        y = jnp.concatenate([dwconv_grid(vg[..., :half], p["conv_w"][:, :half], 2),
                             dwconv_grid(vg[..., half:], p["conv_w"][:, half:], 1)],
                            axis=-1).reshape(b, l, ch)
    return jax.nn.silu(layer_norm(y + p["conv_b"], p["conv_ln_g"], p["conv_ln_b"]))


def group_to_heads(v):
    b, l, _ = v.shape
    return jnp.repeat(v.reshape(b, l, SSD_GROUPS, SSD_STATE), SSD_HEADS // SSD_GROUPS, axis=2)


def ssd_scan_inputs(xbc_raw, dt_raw, p):
    b, l, ch = xbc_raw.shape
    xbc = jax.nn.silu(dwconv_seq(xbc_raw, p["ssd_conv_w"][:, :ch]) + p["ssd_conv_b"][:ch])
    xs = xbc[..., :D_SSD].reshape(b, l, SSD_HEADS, SSD_HEADDIM)
    bm = group_to_heads(xbc[..., D_SSD:D_SSD + D_BC])
    cm = group_to_heads(xbc[..., D_SSD + D_BC:]) if ch == D_XBC else None
    dt = jax.nn.softplus(dt_raw.astype(jnp.float32).reshape(b, l, 2, SSD_HEADS)
                         + p["dt_bias"].astype(jnp.float32))
    return xs, bm, cm, dt


def flip_seq(t):
    return jnp.flip(t, axis=1)


def ssd_chunked(xs, dt, a, bm, cm, h0):
    b, l, nh, hp = xs.shape
    n = bm.shape[-1]
    nc = l // SSD_CHUNK
    a_cum = jnp.cumsum((dt * a).reshape(b, nc, SSD_CHUNK, nh), axis=2)
    xdt = (xs * dt[..., None]).reshape(b, nc, SSD_CHUNK, nh, hp)
    bc = bm.reshape(b, nc, SSD_CHUNK, nh, n)
    cc = cm.reshape(b, nc, SSD_CHUNK, nh, n)
    causal = jnp.tril(jnp.ones((SSD_CHUNK, SSD_CHUNK), dtype=bool))
    seg = a_cum[:, :, :, None, :] - a_cum[:, :, None, :, :]
    decay_ls = jnp.exp(jnp.where(causal[None, None, :, :, None], seg, -jnp.inf))
    scores = jnp.einsum("bclhn,bcshn->bclsh", cc, bc) * decay_ls
    y_diag = jnp.einsum("bclsh,bcshp->bclhp", scores, xdt)
    decay_to_end = jnp.exp(a_cum[:, :, -1:, :] - a_cum)
    chunk_states = jnp.einsum("bclh,bclhn,bclhp->bchpn", decay_to_end, bc, xdt)
    chunk_decay = jnp.exp(a_cum[:, :, -1, :])

    def step(h, inp):
        s, d = inp
        return h * d[:, :, None, None] + s, h

    _, h_prev = lax.scan(step, h0.astype(chunk_states.dtype),
                         (jnp.swapaxes(chunk_states, 0, 1), jnp.swapaxes(chunk_decay, 0, 1)))
    h_prev = jnp.swapaxes(h_prev, 0, 1)
    y_off = jnp.einsum("bclhn,bchpn,bclh->bclhp", cc, h_prev, jnp.exp(a_cum))
    return (y_diag + y_off).reshape(b, l, nh, hp)


def ssd_final_state(xs, dt, a, bm):
    la = jnp.cumsum(dt * a, axis=1)
    w = jnp.exp(la[:, -1:, :] - la) * dt
    return jnp.einsum("blh,blhn,blhp->bhpn", w, bm, xs)


def context_states(xs, bm, dt, a):
    hf = ssd_final_state(xs, dt[:, :, 0], a[0], bm)
    hb = ssd_final_state(flip_seq(xs), flip_seq(dt[:, :, 1]), a[1], flip_seq(bm))
    return hf, hb


def mixer_output(u, gate, z, xs, bm, cm, dt, a, h0_f, h0_b, p, rows):
    conv_out = conformer_conv(u, gate, p, rows)
    y_f = ssd_chunked(xs, dt[:, :, 0], a[0], bm, cm, h0_f)
    y_b = flip_seq(ssd_chunked(flip_seq(xs), flip_seq(dt[:, :, 1]), a[1],
                               flip_seq(bm), flip_seq(cm), h0_b))
    y = y_f + y_b + p["d_skip"][:, None] * xs
    b, l = xs.shape[:2]
    ssd_out = rms_norm(y.reshape(b, l, D_SSD) * jax.nn.silu(z), p["ssd_norm_g"])
    return jnp.concatenate([conv_out, ssd_out], axis=-1) @ p["w_out"]


def hier_moe(h, p):
    b, l, d = h.shape
    t = h.reshape(b * l, d)
    g_logits = (t @ p["w_rg"] + p["b_rg"]).astype(jnp.float32)
    g_sel = jnp.argmax(g_logits, axis=-1)
    g_prob = jnp.take_along_axis(jax.nn.softmax(g_logits, axis=-1), g_sel[:, None], axis=-1)
    e_logits = (t @ p["w_re"] + p["b_re"]).astype(jnp.float32).reshape(-1, MOE_GROUPS, MOE_PER_GROUP)
    e_logits = jnp.take_along_axis(e_logits, g_sel[:, None, None], axis=1)[:, 0]
    top_v, top_i = lax.top_k(jax.nn.softmax(e_logits, axis=-1), MOE_TOPK)
    w_sel = g_prob * top_v / jnp.sum(top_v, axis=-1, keepdims=True)
    e_id = g_sel[:, None] * MOE_PER_GROUP + top_i
    comb = jnp.einsum("tk,tke->et", w_sel, jax.nn.one_hot(e_id, N_EXPERTS, dtype=jnp.float32))

    def expert(acc, ew):
        wg, wu, wd, cw = ew
        hid = jax.nn.silu(t @ wg) * (t @ wu)
        return acc + (cw[:, None] * (hid @ wd)).astype(acc.dtype), None

    out, _ = lax.scan(expert, jnp.zeros(t.shape, jnp.float32),
                      (p["w_gate"], p["w_up"], p["w_down"], comb))
    return out.reshape(b, l, d)


def setup_inputs(seed: int = 0) -> dict:
    key = jax.random.key(seed)
    ks = iter(jax.random.split(key, 40))
    f32 = jnp.float32

    def nrm(shape, scale):
        return scale * jax.random.normal(next(ks), shape, f32)

    def gain(shape):
        return 1.0 + nrm(shape, 0.02)

    dt0 = jnp.exp(jax.random.uniform(next(ks), (DEPTH, 2, SSD_HEADS), f32,
                                     math.log(1e-3), math.log(1e-1)))
    return {
        "x": nrm((BATCH, SEQ, D_MODEL), 1.0),
        "c": nrm((BATCH, D_MODEL), 1.0),
        "ctx": nrm((BATCH, CTX_LEN, D_MODEL), 1.0),
        "c_ctx": nrm((D_MODEL,), 1.0),
        "w_ada": nrm((DEPTH, D_MODEL, 6 * D_MODEL), 0.5 * D_MODEL ** -0.5),
        "b_ada": nrm((DEPTH, 6 * D_MODEL), 0.02),
        "g_mix": gain((DEPTH, D_MODEL)),
        "g_ffn": gain((DEPTH, D_MODEL)),
        "w_in": nrm((DEPTH, D_MODEL, D_IN), D_MODEL ** -0.5),
        "conv_w": nrm((DEPTH, CONV_K, D_CONV), CONV_K ** -0.5),
        "conv_b": nrm((DEPTH, D_CONV), 0.02),
        "conv_ln_g": gain((DEPTH, D_CONV)),
        "conv_ln_b": nrm((DEPTH, D_CONV), 0.02),
        "ssd_conv_w": nrm((DEPTH, SSD_CONV_K, D_XBC), SSD_CONV_K ** -0.5),
        "ssd_conv_b": nrm((DEPTH, D_XBC), 0.02),
        "dt_bias": dt0 + jnp.log(-jnp.expm1(-dt0)),
        "a_log": jnp.log(jax.random.uniform(next(ks), (DEPTH, 2, SSD_HEADS), f32, 1.0, 16.0)),
        "d_skip": gain((DEPTH, SSD_HEADS)),
        "ssd_norm_g": gain((DEPTH, D_SSD)),
        "w_out": nrm((DEPTH, D_MIX, D_MODEL), D_MIX ** -0.5),
        "w_router_group": nrm((DEPTH, D_MODEL, MOE_GROUPS), D_MODEL ** -0.5),
        "b_router_group": nrm((DEPTH, MOE_GROUPS), 0.01),
        "w_router_expert": nrm((DEPTH, D_MODEL, N_EXPERTS), D_MODEL ** -0.5),
        "b_router_expert": nrm((DEPTH, N_EXPERTS), 0.01),
        "w_gate": nrm((DEPTH, N_EXPERTS, D_MODEL, EXPERT_FF), D_MODEL ** -0.5),
        "w_up": nrm((DEPTH, N_EXPERTS, D_MODEL, EXPERT_FF), D_MODEL ** -0.5),
        "w_down": nrm((DEPTH, N_EXPERTS, EXPERT_FF, D_MODEL), EXPERT_FF ** -0.5),
        "g_final": gain((D_MODEL,)),
    }


def reference(x, c, ctx, c_ctx, w_ada, b_ada, g_mix, g_ffn, w_in, conv_w, conv_b, conv_ln_g,
              conv_ln_b, ssd_conv_w, ssd_conv_b, dt_bias, a_log, d_skip, ssd_norm_g, w_out,
              w_router_group, b_router_group, w_router_expert, b_router_expert,
              w_gate, w_up, w_down, g_final):
    rows = x.shape[1] // GRID_W
    h_x, h_c = x, ctx
    lo = 2 * D_CONV + D_SSD
    for l in range(DEPTH):
        last = l == DEPTH - 1
        p = {"w_in": w_in[l], "conv_w": conv_w[l], "conv_b": conv_b[l], "conv_ln_g": conv_ln_g[l],
             "conv_ln_b": conv_ln_b[l], "ssd_conv_w": ssd_conv_w[l], "ssd_conv_b": ssd_conv_b[l],
             "dt_bias": dt_bias[l], "d_skip": d_skip[l], "ssd_norm_g": ssd_norm_g[l],
             "w_out": w_out[l], "w_rg": w_router_group[l], "b_rg": b_router_group[l],
             "w_re": w_router_expert[l], "b_re": b_router_expert[l],
             "w_gate": w_gate[l], "w_up": w_up[l], "w_down": w_down[l]}
        a = -jnp.exp(a_log[l].astype(jnp.float32))
        sh1, sc1, gt1, sh2, sc2, gt2 = [m[:, None, :] for m in ada_mod(c, w_ada[l], b_ada[l], 6)]

        if last:
            csh1, csc1 = ada_mod(c_ctx, w_ada[l], b_ada[l], 2)
            hc = modulate(rms_norm(h_c, g_mix[l]), csh1, csc1)
            w_xb_dt = jnp.concatenate([p["w_in"][:, lo:lo + D_SSD + D_BC], p["w_in"][:, lo + D_XBC:]], axis=1)
            proj_c = hc @ w_xb_dt
            xs_c, bm_c, _, dt_c = ssd_scan_inputs(proj_c[..., :D_SSD + D_BC], proj_c[..., D_SSD + D_BC:], p)
        else:
            csh1, csc1, cgt1, csh2, csc2, cgt2 = ada_mod(c_ctx, w_ada[l], b_ada[l], 6)
            hc = modulate(rms_norm(h_c, g_mix[l]), csh1, csc1)
            uc, gc, zc, xbc_c, dtr_c = split_proj(hc @ p["w_in"])
            xs_c, bm_c, cm_c, dt_c = ssd_scan_inputs(xbc_c, dtr_c, p)
        hf0, hb0 = context_states(xs_c, bm_c, dt_c, a)

        hx = modulate(rms_norm(h_x, g_mix[l]), sh1, sc1)
        u, gate, z, xbc, dt_raw = split_proj(hx @ p["w_in"])
        xs, bm, cm, dt = ssd_scan_inputs(xbc, dt_raw, p)
        h_x = h_x + gt1 * mixer_output(u, gate, z, xs, bm, cm, dt, a, hf0, hb0, p, rows)
        h_x = h_x + gt2 * hier_moe(modulate(rms_norm(h_x, g_ffn[l]), sh2, sc2), p)

        if not last:
            zero = jnp.zeros_like(hf0)
            h_c = h_c + cgt1 * mixer_output(uc, gc, zc, xs_c, bm_c, cm_c, dt_c, a, zero, zero, p, None)
            h_c = h_c + cgt2 * hier_moe(modulate(rms_norm(h_c, g_ffn[l]), csh2, csc2), p)

    return rms_norm(h_x, g_final).astype(x.dtype)
```

```python
import numpy as np
import concourse.bass as bass
import concourse.mybir as mybir
from concourse.bass_utils import run_bass_kernel_spmd

F32 = mybir.dt.float32
BF16 = mybir.dt.bfloat16
AF = mybir.ActivationFunctionType
ALU = mybir.AluOpType
AX = mybir.AxisListType

D = 1024
DIN = 2576
NE = 16
FF = 256
RMS_EPS = 1e-6
LN_EPS = 1e-5
NEG = -1.0e30


class Ref:
    __slots__ = ("ap", "buf")

    def __init__(self, ap, buf):
        self.ap = ap
        self.buf = buf

    def __getitem__(self, k):
        return Ref(self.ap[k], self.buf)

    def rearrange(self, s, **kw):
        return Ref(self.ap.rearrange(s, **kw), self.buf)

    def unsqueeze(self, a):
        return Ref(self.ap.unsqueeze(a), self.buf)

    def to_broadcast(self, shape):
        return Ref(self.ap.to_broadcast(shape), self.buf)


class Buf:
    def __init__(self, handle, name):
        self.t = handle
        self.name = name
        self.last_w = None
        self.readers = {}
        self.dsem = None

    def __getitem__(self, k):
        return Ref(self.t[k], self)


class Sched:
    ENG = ("pe", "act", "dve", "pool", "sp")

    def __init__(self, nc):
        self.nc = nc
        self.eng = {"pe": nc.tensor, "act": nc.scalar, "dve": nc.vector, "pool": nc.gpsimd, "sp": nc.sync}
        self.sems = {}
        self.cnt = {}
        for e in self.ENG:
            self.sems[e] = nc.alloc_semaphore("s_" + e)
            self.cnt[e] = 0
        self.known = {e: {} for e in self.ENG}
        self.free_dsems = []
        self.n_dsem = 0
        self.ninst = 0

    def _get_dsem(self, buf):
        if buf.dsem is None:
            if self.free_dsems:
                buf.dsem = self.free_dsems.pop()
            else:
                key = "d%d" % self.n_dsem
                self.n_dsem += 1
                self.sems[key] = self.nc.alloc_semaphore("s_" + key)
                self.cnt[key] = 0
                buf.dsem = key
        return buf.dsem

    def release(self, bufs):
        for b in bufs:
            if b.dsem is not None:
                self.free_dsems.append(b.dsem)
                b.dsem = None

    def _wait(self, e, need):
        eng = self.eng[e]
        kn = self.known[e]
        for k, v in need.items():
            if kn.get(k, 0) < v:
                eng.wait_ge(self.sems[k], v)
                kn[k] = v
                self.ninst += 1

    def _deps(self, e, reads, writes):
        need = {}

        def add(ev):
            if ev is None:
                return
            k, v = ev
            if e == "pe" and k == "pe":
                return
            if need.get(k, 0) < v:
                need[k] = v
        for b in reads:
            add(b.last_w)
        for b in writes:
            add(b.last_w)
            for k, v in b.readers.items():
                add((k, v))
        return need

    def op(self, e, name, _ni=False, **kw):
        reads, writes, args = [], [], {}
        for k, v in kw.items():
            if isinstance(v, Ref):
                (writes if k in ("out", "accum_out") else reads).append(v.buf)
                args[k] = v.ap
            else:
                args[k] = v
        self._wait(e, self._deps(e, reads, writes))
        ins = getattr(self.eng[e], name)(**args)
        self.ninst += 1
        if _ni and e == "pe" and name == "matmul" and kw.get("stop") is False:
            ev = (e, self.cnt[e] + 1)
        else:
            ins.then_inc(self.sems[e], 1)
            self.cnt[e] += 1
            ev = (e, self.cnt[e])
        for b in reads:
            if b not in writes:
                b.readers[e] = ev[1]
        for b in writes:
            b.last_w = ev
            b.readers = {}
        return ins

    def dma(self, q, out, in_):
        reads, writes = [], []
        o = out
        i = in_
        if isinstance(out, Ref):
            writes.append(out.buf)
            o = out.ap
        if isinstance(in_, Ref):
            reads.append(in_.buf)
            i = in_.ap
        self._wait(q, self._deps(q, reads, writes))
        tb = (writes + reads)[0]
        key = self._get_dsem(tb)
        self.eng[q].dma_start(out=o, in_=i).then_inc(self.sems[key], 16)
        self.cnt[key] += 16
        self.ninst += 1
        ev = (key, self.cnt[key])
        for b in reads:
            b.readers[key] = ev[1]
        for b in writes:
            b.last_w = ev
            b.readers = {}

    def collective(self, ins_ap, outs_ap, ncores):
        if "cc" not in self.sems:
            self.sems["cc"] = self.nc.alloc_semaphore("s_cc")
            self.cnt["cc"] = 0
        self.barrier()
        ins = self.nc.gpsimd.collective_compute("AllGather", ALU.bypass, replica_groups=[list(range(ncores))],
                                                ins=[ins_ap.opt()], outs=[outs_ap.opt()])
        ins.then_inc(self.sems["cc"], 1)
        self.cnt["cc"] += 1
        self.ninst += 1
        self.barrier()

    def barrier(self):
        for e in self.ENG:
            need = {k: v for k, v in self.cnt.items() if v > 0 and k != e}
            self._wait(e, need)


def build(L, CTX, depth=2, dbg=(), stop_after=None, ncores=1):
    multi = ncores > 1
    nc = bass.Bass("TRN2", target_bir_lowering=False)
    S = Sched(nc)
    ROWS = L // 64
    dbg = set(dbg)

    def din(name, shape, dt=F32):
        return nc.dram_tensor(name, list(shape), dt, kind="ExternalInput").ap()

    def dscr(name, shape, dt=F32):
        kind = "ExternalOutput" if name in dbg else "Internal"
        return nc.dram_tensor(name, list(shape), dt, kind=kind).ap()

    x_in = din("x", [L, D])
    ctx_in = din("ctx", [CTX, D])
    cvT = din("cvT", [D, 2])
    w_ada = din("w_ada", [depth, D, 6 * D])
    b_ada = din("b_ada", [depth, 6 * D])
    g_mix = din("g_mix", [depth, D])
    g_ffn = din("g_ffn", [depth, D])
    w_in = din("w_in", [depth, D, DIN])
    conv_wT = din("conv_wT", [depth, 128, 4, 31])
    conv_pc = din("conv_pc", [depth, 128, 3, 4])
    sconv_wT = din("sconv_wT", [depth, 128, 8, 5])
    sconv_b = din("sconv_b", [depth, 128, 8])
    dt_bias = din("dt_bias", [depth, 16])
    a_log = din("a_log", [depth, 16])
    d_skip = din("d_skip", [depth, 8])
    ssd_g = din("ssd_norm_g", [depth, 512])
    w_out = din("w_out", [depth, D, D])
    w_r = din("w_r", [depth, D, 20])
    b_r = din("b_r", [depth, 20])
    w_gate = din("w_gate", [depth, NE, D, FF])
    w_up = din("w_up", [depth, NE, D, FF])
    w_down = din("w_down", [depth, NE, FF, D])
    g_final = din("g_final", [1, D])
    msk_in = din("msk", [1, 32])
    out_d = nc.dram_tensor("out", [L, D], F32, kind="ExternalOutput").ap()

    modd = dscr("modd", [depth, 2, 6 * D])
    wgu_bf = dscr("wgu_bf", [NE, 128, 8, 2 * FF], BF16)
    wd_bf = dscr("wd_bf", [NE, 128, 2, D], BF16)
    W1 = 3872
    W2 = 1040
    send1 = dscr("send1", [128, W1], BF16)
    recv1 = dscr("recv1", [max(ncores, 1) * 128, W1], BF16)
    haloL = dscr("haloL", [128, W1], BF16)
    haloR = dscr("haloR", [128, W1], BF16)
    send2 = dscr("send2", [128, W2])
    recv2 = dscr("recv2", [max(ncores, 1) * 128, W2])
    streams = {}
    for sname, T in (("lat", L), ("ctx", CTX)):
        streams[sname] = dict(
            T=T,
            h=dscr(sname + "_h", [T, D]),
            vT=dscr(sname + "_vT", [512, T], BF16),
            xbcT=dscr(sname + "_xbcT", [1024, T], BF16),
            sz=dscr(sname + "_sz", [T, 512]),
            dt=dscr(sname + "_dt", [T, 16]),
            coT=dscr(sname + "_coT", [512, T], BF16),
            xcT=dscr(sname + "_xcT", [512, T], BF16),
            xbtok=dscr(sname + "_xbtok", [T, 768], BF16),
            Hbd=dscr(sname + "_Hbd", [T // 128, 128, 512], BF16),
        )

    class Pool:
        uid = [0]

        def __init__(self):
            self.stack = []
            self.bufs = []

        def _nm(self, name):
            Pool.uid[0] += 1
            return "%s_%d" % (name, Pool.uid[0])

        def sb(self, name, shape, dt=F32):
            g = nc.sbuf_tensor(self._nm(name), list(shape), dt)
            h = g.__enter__()
            self.stack.append(g)
            b = Buf(h, name)
            self.bufs.append(b)
            return b

        def ps(self, name, shape, dt=F32):
            g = nc.psum_tensor(self._nm(name), list(shape), dt)
            h = g.__enter__()
            self.stack.append(g)
            b = Buf(h, name)
            self.bufs.append(b)
            return b

        def close(self):
            S.barrier()
            S.release(self.bufs)
            for g in reversed(self.stack):
                g.__exit__(None, None, None)
            self.stack = []
            self.bufs = []

    CP = Pool()
    ident_f = CP.sb("ident_f", [128, 128])
    ident_b = CP.sb("ident_b", [128, 128], BF16)
    ones_f = CP.sb("ones_f", [128, 128])
    Umat = CP.sb("Umat", [128, 128])
    Vmat = CP.sb("Vmat", [128, 128])
    mS = CP.sb("mS", [128, 128])
    mSp = CP.sb("mSp", [128, 128])
    sel = CP.sb("sel", [16, NE, 128])
    Hf = CP.sb("Hf", [128, 512])
    Hb = CP.sb("Hb", [128, 512])
    hf0 = CP.sb("hf0", [128, 512])
    hb0 = CP.sb("hb0", [128, 512])
    Dacc = CP.sb("Dacc", [128, 16])
    msk = CP.sb("msk", [128, 32])
    omsk = CP.sb("omsk", [128, 32])

    def memset(e, ref, val):
        reads, writes = [], [ref.buf]
        S._wait(e, S._deps(e, reads, writes))
        ins = S.eng[e].memset(ref.ap, val)
        ins.then_inc(S.sems[e], 1)
        S.cnt[e] += 1
        S.ninst += 1
        ref.buf.last_w = (e, S.cnt[e])
        ref.buf.readers = {}

    def asel(ref, pattern, cm, cmp, fill, base=0):
        S.op("pool", "affine_select", out=ref, in_=ref, pattern=pattern, compare_op=cmp, fill=fill,
             base=base, channel_multiplier=cm)

    def cast(e, out, in_):
        if e == "act":
            S.op("act", "activation", out=out, in_=in_, func=AF.Copy)
        else:
            S.op(e, "tensor_copy", out=out, in_=in_)

    memset("pool", ident_f[:], 0.0)
    asel(ident_f[:], [[-1, 128]], 1, ALU.not_equal, 1.0)
    S.op("dve", "tensor_copy", out=ident_b[:], in_=ident_f[:])
    memset("pool", ones_f[:], 1.0)
    memset("pool", Umat[:], 1.0)
    asel(Umat[:], [[1, 128]], -1, ALU.is_ge, 0.0)
    memset("pool", Vmat[:], 1.0)
    asel(Vmat[:], [[-1, 128]], 1, ALU.is_ge, 0.0)
    memset("pool", mS[:], 1.0)
    asel(mS[:], [[-1, 128]], 1, ALU.is_gt, 0.0)
    memset("pool", mSp[:], 1.0)
    asel(mSp[:], [[1, 128]], -1, ALU.is_gt, 0.0)
    memset("pool", sel[:], 0.0)
    asel(sel[:], [[-1, NE], [0, 128]], 1, ALU.not_equal, 1.0)
    NEGB = -30000.0
    negU = CP.sb("negU", [128, 128], BF16)
    negV = CP.sb("negV", [128, 128], BF16)
    negtmp = CP.sb("negtmp", [128, 128])
    memset("pool", negtmp[:], NEGB)
    asel(negtmp[:], [[-1, 128]], 1, ALU.is_gt, 0.0)
    S.op("dve", "tensor_copy", out=negU[:], in_=negtmp[:])
    memset("pool", negtmp[:], NEGB)
    asel(negtmp[:], [[1, 128]], -1, ALU.is_gt, 0.0)
    S.op("dve", "tensor_copy", out=negV[:], in_=negtmp[:])
    S.dma("sp", msk[:], msk_in[0:1, :].partition_broadcast(128))
    S.op("dve", "tensor_scalar", out=omsk[:], in0=msk[:], scalar1=-1.0, scalar2=1.0, op0=ALU.mult, op1=ALU.add)

    def rstd_from_ssq(P, ssq, n, inv_n, eps):
        S.op("act", "activation", out=ssq[:, 0:n], in_=ssq[:, 0:n], func=AF.Ln, bias=eps, scale=inv_n)
        S.op("act", "activation", out=ssq[:, 0:n], in_=ssq[:, 0:n], func=AF.Exp, scale=-0.5)

    def load_mod_bcast(P, l, s_idx, which, name):
        b = P.sb(name, [128, D])
        S.dma("sp", b[:], modd[l, s_idx:s_idx + 1, which * D:(which + 1) * D].partition_broadcast(128))
        return b

    def load_vec_bcast(P, ap2d, n, name):
        b = P.sb(name, [128, n])
        S.dma("sp", b[:], ap2d.partition_broadcast(128))
        return b

    def phase_copy_in():
        P = Pool()
        for src, dst, T in ((x_in, streams["lat"]["h"], L), (ctx_in, streams["ctx"]["h"], CTX)):
            nt = T // 128
            step = min(nt, 8)
            bufs = [P.sb("cp", [128, step, D]) for _ in range(2)]
            for i, t0 in enumerate(range(0, nt, step)):
                b = bufs[i % 2]
                sv = src[t0 * 128:(t0 + step) * 128, :].rearrange("(j p) d -> p j d", p=128)
                dv = dst[t0 * 128:(t0 + step) * 128, :].rearrange("(j p) d -> p j d", p=128)
                S.dma("sp", b[:], sv)
                S.dma("pool", dv, b[:])
        P.close()

    def phase_ada(l):
        P = Pool()
        cv = P.sb("cv", [128, 8, 2])
        S.dma("sp", cv[:], cvT.rearrange("(c p) s -> p c s", p=128))
        S.op("act", "activation", out=cv[:], in_=cv[:], func=AF.Silu)
        bb = P.sb("bb", [2, 6 * D])
        S.dma("sp", bb[:], b_ada[l:l + 1, :].partition_broadcast(2))
        mo = P.sb("mo", [2, 6 * D])
        wts = [P.sb("wada", [128, 8, 512]) for _ in range(2)]
        pss = [P.ps("psa", [128, 512]) for _ in range(2)]
        for cb in range(12):
            wt = wts[cb % 2]
            ps = pss[cb % 2]
            S.dma("sp", wt[:], w_ada[l, :, cb * 512:(cb + 1) * 512].rearrange("(c p) n -> p c n", p=128))
            for kc in range(8):
                S.op("pe", "matmul", out=ps[0:2, :], lhsT=cv[:, kc, :], rhs=wt[:, kc, :], start=(kc == 0), stop=(kc == 7))
            S.op("dve", "tensor_tensor", out=mo[:, cb * 512:(cb + 1) * 512], in0=ps[0:2, :], in1=bb[:, cb * 512:(cb + 1) * 512], op=ALU.add)
        S.dma("pool", modd[l], mo[:])
        P.close()

    def phase_wprep(l):
        P = Pool()
        st = [P.sb("wst", [128, 8, FF]) for _ in range(2)]
        ob = [P.sb("wob", [128, 8, 2 * FF], BF16) for _ in range(2)]
        st2 = [P.sb("wst2", [128, 2, D]) for _ in range(2)]
        ob2 = [P.sb("wob2", [128, 2, D], BF16) for _ in range(2)]
        k = 0
        for e in range(NE):
            o = ob[e % 2]
            for j, w in enumerate((w_gate, w_up)):
                s_ = st[k % 2]
                k += 1
                S.dma("sp", s_[:], w[l, e].rearrange("(c p) f -> p c f", p=128))
                cast("act" if j == 0 else "dve", o[:, :, j * FF:(j + 1) * FF], s_[:])
            S.dma("pool", wgu_bf[e], o[:])
            s2 = st2[e % 2]
            o2 = ob2[e % 2]
            S.dma("sp", s2[:], w_down[l, e].rearrange("(c p) d -> p c d", p=128))
            S.op("dve", "tensor_copy", out=o2[:], in_=s2[:])
            S.dma("pool", wd_bf[e], o2[:])
        P.close()

    def phase_p1(l, sname):
        st = streams[sname]
        T = st["T"]
        s_idx = 0 if sname == "lat" else 1
        TT = min(512, T)
        nsub = TT // 128
        P = Pool()
        G1 = load_mod_bcast(P, l, s_idx, 1, "G1")
        S1 = load_mod_bcast(P, l, s_idx, 0, "S1")
        gm = load_vec_bcast(P, g_mix[l:l + 1, :], D, "gm")
        S.op("dve", "scalar_tensor_tensor", out=G1[:], in0=G1[:], scalar=1.0, in1=gm[:], op0=ALU.add, op1=ALU.mult)
        dtb = load_vec_bcast(P, dt_bias[l:l + 1, :], 16, "dtb")
        wbf = P.sb("wbf", [128, 8, DIN], BF16)
        wst = [P.sb("wst", [128, 8, 368]) for _ in range(2)]
        for i in range(7):
            c0 = i * 368
            s_ = wst[i % 2]
            S.dma("sp", s_[:], w_in[l, :, c0:c0 + 368].rearrange("(c p) n -> p c n", p=128))
            cast("act" if i % 2 else "dve", wbf[:, :, c0:c0 + 368], s_[:])
        ht = [P.sb("ht", [128, nsub, D]) for _ in range(2)]
        junk = P.sb("junk", [128, D])
        ssq = P.sb("ssq", [128, 4])
        hx = P.sb("hx", [128, D], BF16)
        tmp = P.sb("tmp", [128, D])
        hxTs = [P.sb("hxT", [128, 8, TT], BF16) for _ in range(2)]
        pT = P.ps("pT", [128, 8 * 128], BF16)
        psA = [P.ps("psA", [128, 512]) for _ in range(2)]
        psB = [P.ps("psB", [128, 512]) for _ in range(2)]
        psD = P.ps("psD", [128, 16])
        sg = P.sb("sg", [128, 512])
        v_sb = [P.sb("v_sb", [128, 512], BF16) for _ in range(2)]
        xb_sb = [P.sb("xb_sb", [128, 512], BF16) for _ in range(2)]
        sz_sb = P.sb("sz_sb", [128, nsub, 512])
        dt_sb = P.sb("dt_sb", [128, nsub, 16])
        def stage_a(ti):
            t0 = ti * TT
            h_ = ht[ti % 2]
            hxT = hxTs[ti % 2]
            S.dma("sp", h_[:], st["h"][t0:t0 + TT, :].rearrange("(j p) d -> p j d", p=128))
            for j in range(nsub):
                S.op("act", "activation", out=junk[:], in_=h_[:, j, :], func=AF.Square, accum_out=ssq[:, j:j + 1])
            rstd_from_ssq(P, ssq, nsub, 1.0 / D, RMS_EPS)
            for j in range(nsub):
                S.op("dve", "scalar_tensor_tensor", out=tmp[:], in0=h_[:, j, :], scalar=ssq[:, j:j + 1], in1=G1[:], op0=ALU.mult, op1=ALU.mult)
                S.op("dve", "tensor_tensor", out=hx[:], in0=tmp[:], in1=S1[:], op=ALU.add)
                for kc in range(8):
                    S.op("pe", "transpose", out=pT[:, kc * 128:(kc + 1) * 128], in_=hx[:, kc * 128:(kc + 1) * 128], identity=ident_b[:])
                S.op("act", "activation", out=hxT[:, :, j * 128:(j + 1) * 128], in_=pT[:].rearrange("p (c t) -> p c t", c=8), func=AF.Copy)

        def stage_b(ti):
            t0 = ti * TT
            hxT = hxTs[ti % 2]
            for c in range(4):
                pu, pg = psA[c % 2], psB[c % 2]
                for kc in range(8):
                    S.op("pe", "matmul", _ni=True, out=pu[:, 0:TT], lhsT=wbf[:, kc, c * 128:(c + 1) * 128], rhs=hxT[:, kc, :], start=(kc == 0), stop=(kc == 7))
                for kc in range(8):
                    S.op("pe", "matmul", _ni=True, out=pg[:, 0:TT], lhsT=wbf[:, kc, 512 + c * 128:512 + (c + 1) * 128], rhs=hxT[:, kc, :], start=(kc == 0), stop=(kc == 7))
                S.op("act", "activation", out=sg[:, 0:TT], in_=pg[:, 0:TT], func=AF.Sigmoid)
                vb = v_sb[c % 2]
                S.op("dve", "tensor_tensor", out=vb[:, 0:TT], in0=pu[:, 0:TT], in1=sg[:, 0:TT], op=ALU.mult)
                S.dma("pool", st["vT"][c * 128:(c + 1) * 128, t0:t0 + TT], vb[:, 0:TT])
            for c in range(8):
                pu = (psA + psB)[c % 4]
                col = 1536 + c * 128
                for kc in range(8):
                    S.op("pe", "matmul", _ni=True, out=pu[:, 0:TT], lhsT=wbf[:, kc, col:col + 128], rhs=hxT[:, kc, :], start=(kc == 0), stop=(kc == 7))
                xb = xb_sb[c % 2]
                S.op("act", "activation", out=xb[:, 0:TT], in_=pu[:, 0:TT], func=AF.Copy)
                S.dma("pool", st["xbcT"][c * 128:(c + 1) * 128, t0:t0 + TT], xb[:, 0:TT])
            for j in range(nsub):
                pz = psA[j % 2]
                for kc in range(8):
                    S.op("pe", "matmul", _ni=True, out=pz[:], lhsT=hxT[:, kc, j * 128:(j + 1) * 128], rhs=wbf[:, kc, 1024:1536], start=(kc == 0), stop=(kc == 7))
                for kc in range(8):
                    S.op("pe", "matmul", _ni=True, out=psD[:], lhsT=hxT[:, kc, j * 128:(j + 1) * 128], rhs=wbf[:, kc, 2560:2576], start=(kc == 0), stop=(kc == 7))
                S.op("act", "activation", out=sz_sb[:, j, :], in_=pz[:], func=AF.Silu)
                S.op("dve", "tensor_tensor", out=dt_sb[:, j, :], in0=psD[:], in1=dtb[:], op=ALU.add)
            S.op("act", "activation", out=dt_sb[:], in_=dt_sb[:], func=AF.Exp)
            S.op("act", "activation", out=dt_sb[:], in_=dt_sb[:], func=AF.Ln, bias=1.0, scale=1.0)
            S.dma("pool", st["sz"][t0:t0 + TT, :].rearrange("(j p) d -> p j d", p=128), sz_sb[:])
            S.dma("pool", st["dt"][t0:t0 + TT, :].rearrange("(j p) d -> p j d", p=128), dt_sb[:])

        ntile = T // TT
        stage_a(0)
        for ti in range(ntile):
            if ti + 1 < ntile:
                stage_a(ti + 1)
            stage_b(ti)
        P.close()

    def phase_p2(l, sname):
        st = streams[sname]
        T = st["T"]
        grid = sname == "lat"
        P = Pool()
        cw = P.sb("cw", [128, 4, 31])
        S.dma("sp", cw[:], conv_wT[l])
        cpc = P.sb("cpc", [128, 3, 4])
        S.dma("sp", cpc[:], conv_pc[l])
        dg = P.sb("dg", [128, 4, 31, 128], BF16)
        for c in range(4):
            S.op("dve", "tensor_tensor", out=dg[:, c, :, :], in0=ident_f[:].unsqueeze(1).to_broadcast([128, 31, 128]),
                 in1=cw[:, c, :].unsqueeze(2).to_broadcast([128, 31, 128]), op=ALU.mult)
        NT = 512 if grid else T
        acc = [P.sb("acc", [128, NT]) for _ in range(4)]
        sq = [P.sb("sq", [128, NT]) for _ in range(2)]
        pcs = [P.ps("pc", [128, 512]) for _ in range(3)]
        ps1 = P.ps("ps1", [128, 512])
        ps2 = P.ps("ps2", [128, 512])
        mean = P.sb("mean", [128, NT])
        var = P.sb("var", [128, NT])
        tnorm = [P.sb("tnorm", [128, NT]) for _ in range(2)]
        co = [P.sb("co", [128, NT], BF16) for _ in range(2)]
        if grid:
            RT = 8
            wcol = [P.sb("wcol", [128, RT, 94], BF16) for _ in range(3)]
            for w_ in wcol:
                memset("dve", w_[:], 0.0)
            wrow = [P.sb("wrow", [128, RT + 30, 64], BF16) for _ in range(3)]
        else:
            wseq = [P.sb("wseq", [128, T + 30], BF16) for _ in range(2)]
        ntiles = T // NT
        k = 0
        kr = 0
        ip = 0
        for ti in range(ntiles):
            t0 = ti * NT
            for c in range(4):
                a_ = acc[c]
                pc_ = pcs[ip % 3]
                ip += 1
                if grid and c < 2:
                    w_ = wcol[k % 3]
                    k += 1
                    S.dma("sp", w_[:, :, 15:79], st["vT"][c * 128:(c + 1) * 128, t0:t0 + NT].rearrange("p (r w) -> p r w", w=64))
                    for j in range(31):
                        S.op("pe", "matmul", out=pc_[:].rearrange("p (r w) -> p r w", w=64), lhsT=dg[:, c, j, :], rhs=w_[:, :, j:j + 64],
                             start=(j == 0), stop=(j == 30))
                else:
                    if grid:
                        r0 = t0 // 64
                        w_ = wrow[kr % 3]
                        kr += 1
                        lo_r, hi_r = r0 - 15, r0 + RT + 15
                        vlo, vhi = max(lo_r, 0), min(hi_r, ROWS)
                        if (vlo > lo_r or vhi < hi_r) and not multi:
                            memset("dve", w_[:], 0.0)
                        S.dma("sp", w_[:, vlo - lo_r:vhi - lo_r, :], st["vT"][c * 128:(c + 1) * 128, vlo * 64:vhi * 64].rearrange("p (r w) -> p r w", w=64))
                        if multi and lo_r < 0:
                            n_ = -lo_r
                            src = haloL[:, 1920 + (c - 2) * 960:1920 + (c - 1) * 960].rearrange("p (r w) -> p r w", w=64)
                            S.dma("sp", w_[:, 0:n_, :], src[:, 15 - n_:15, :])
                        if multi and hi_r > ROWS:
                            n_ = hi_r - ROWS
                            src = haloR[:, (c - 2) * 960:(c - 1) * 960].rearrange("p (r w) -> p r w", w=64)
                            S.dma("sp", w_[:, ROWS - lo_r:ROWS - lo_r + n_, :], src[:, 0:n_, :])
                        wf = w_[:].rearrange("p r w -> p (r w)")
                        step = 64
                    else:
                        w_ = wseq[k % 2]
                        k += 1
                        memset("dve", w_[:], 0.0)
                        S.dma("sp", w_[:, 15:15 + T], st["vT"][c * 128:(c + 1) * 128, :])
                        wf = w_[:]
                        step = 1
                    for j in range(31):
                        S.op("pe", "matmul", out=pc_[:, 0:NT], lhsT=dg[:, c, j, :], rhs=wf[:, j * step:j * step + NT], start=(j == 0), stop=(j == 30))
                S.op("dve", "tensor_scalar", out=a_[:], in0=pc_[:, 0:NT], scalar1=cpc[:, 0, c:c + 1], scalar2=None, op0=ALU.add)
            for c in range(4):
                S.op("pe", "matmul", out=ps1[:, 0:NT], lhsT=ones_f[:], rhs=acc[c][:], start=(c == 0), stop=(c == 3))
            for c in range(4):
                q_ = sq[c % 2]
                S.op("act", "activation", out=q_[:], in_=acc[c][:], func=AF.Square)
                S.op("pe", "matmul", out=ps2[:, 0:NT], lhsT=ones_f[:], rhs=q_[:], start=(c == 0), stop=(c == 3))
            S.op("act", "activation", out=mean[:], in_=ps1[:, 0:NT], func=AF.Copy, scale=1.0 / 512)
            S.op("dve", "tensor_tensor", out=var[:], in0=mean[:], in1=mean[:], op=ALU.mult)
            S.op("dve", "scalar_tensor_tensor", out=var[:], in0=ps2[:, 0:NT], scalar=1.0 / 512, in1=var[:], op0=ALU.mult, op1=ALU.subtract)
            S.op("act", "activation", out=var[:], in_=var[:], func=AF.Ln, bias=LN_EPS, scale=1.0)
            S.op("act", "activation", out=var[:], in_=var[:], func=AF.Exp, scale=-0.5)
            for c in range(4):
                tn = tnorm[c % 2]
                S.op("dve", "tensor_tensor", out=tn[:], in0=acc[c][:], in1=mean[:], op=ALU.subtract)
                S.op("dve", "tensor_tensor", out=tn[:], in0=tn[:], in1=var[:], op=ALU.mult)
                o = co[c % 2]
                S.op("act", "activation", out=o[:], in_=tn[:], func=AF.Silu, bias=cpc[:, 2, c:c + 1], scale=cpc[:, 1, c:c + 1])
                S.dma("pool", st["coT"][c * 128:(c + 1) * 128, t0:t0 + NT], o[:])
        P.close()

    def phase_p2b(l, sname):
        st = streams[sname]
        T = st["T"]
        TT = min(512, T)
        nsub = TT // 128
        P = Pool()
        sw = P.sb("sw", [128, 8, 5])
        S.dma("sp", sw[:], sconv_wT[l])
        sb_ = P.sb("sb_", [128, 8])
        S.dma("sp", sb_[:], sconv_b[l])
        dg = P.sb("dg5", [128, 8, 5, 128], BF16)
        for c in range(8):
            S.op("dve", "tensor_tensor", out=dg[:, c, :, :], in0=ident_f[:].unsqueeze(1).to_broadcast([128, 5, 128]),
                 in1=sw[:, c, :].unsqueeze(2).to_broadcast([128, 5, 128]), op=ALU.mult)
        win = [P.sb("win", [128, 8, TT + 4], BF16) for _ in range(2)]
        xc = [P.sb("xc", [128, 8, TT], BF16) for _ in range(2)]
        pcs = [P.ps("pc", [128, 512]) for _ in range(3)]
        pT = P.ps("pT", [128, 768], BF16)
        tok = [P.sb("tok", [128, 768], BF16) for _ in range(2)]
        ntiles = T // TT
        q = 0
        ip = 0
        for ti in range(ntiles):
            t0 = ti * TT
            w_ = win[ti % 2]
            lo, hi = t0 - 2, t0 + TT + 2
            vlo, vhi = max(lo, 0), min(hi, T)
            use_halo = multi and sname == "lat"
            if (vlo > lo or vhi < hi) and not use_halo:
                memset("dve", w_[:], 0.0)
            S.dma("sp", w_[:, :, vlo - lo:vhi - lo], st["xbcT"][:, vlo:vhi].rearrange("(c p) t -> p c t", p=128))
            if use_halo and lo < 0:
                S.dma("sp", w_[:, :, 0:2], haloL[:, 3840:3872].rearrange("p (c t) -> p c t", t=4)[:, :, 2:4])
            if use_halo and hi > T:
                S.dma("sp", w_[:, :, TT + 2:TT + 4], haloR[:, 3840:3872].rearrange("p (c t) -> p c t", t=4)[:, :, 0:2])
            x_ = xc[ti % 2]
            for c in range(8):
                pc_ = pcs[ip % 3]
                ip += 1
                for j in range(5):
                    S.op("pe", "matmul", out=pc_[:, 0:TT], lhsT=dg[:, c, j, :], rhs=w_[:, c, j:j + TT], start=(j == 0), stop=(j == 4))
                S.op("act", "activation", out=x_[:, c, :], in_=pc_[:, 0:TT], func=AF.Silu, bias=sb_[:, c:c + 1], scale=1.0)
            S.dma("pool", st["xcT"][:, t0:t0 + TT].rearrange("(c p) t -> p c t", p=128), x_[:, 4:8, :])
            for j in range(nsub):
                for c in range(6):
                    S.op("pe", "transpose", out=pT[:, c * 128:(c + 1) * 128], in_=x_[:, c, j * 128:(j + 1) * 128], identity=ident_b[:])
                tk = tok[q % 2]
                q += 1
                S.op("act", "activation", out=tk[:], in_=pT[:], func=AF.Copy)
                S.dma("pool", st["xbtok"][t0 + j * 128:t0 + (j + 1) * 128, :], tk[:])
        P.close()

    def ssd_consts(P, l):
        al = load_vec_bcast(P, a_log[l:l + 1, :], 16, "al")
        S.op("act", "activation", out=al[:], in_=al[:], func=AF.Exp)
        S.op("dve", "tensor_scalar", out=al[:], in0=al[:], scalar1=-1.0, scalar2=None, op0=ALU.mult)
        return al

    def chunk_decays(P, W, dt_c, a_bc, cum_ps):
        S.op("dve", "tensor_tensor", out=W["dta"][:], in0=dt_c[:], in1=a_bc[:], op=ALU.mult)
        S.op("pe", "matmul", out=cum_ps[:, 0:8], lhsT=Umat[:], rhs=W["dta"][:, 0:8], start=True, stop=True)
        S.op("pe", "matmul", out=cum_ps[:, 8:16], lhsT=Vmat[:], rhs=W["dta"][:, 8:16], start=True, stop=True)
        S.op("pe", "matmul", out=cum_ps[:, 16:32], lhsT=ones_f[:], rhs=W["dta"][:], start=True, stop=True)
        S.op("act", "activation", out=W["cum"][:], in_=cum_ps[:, 0:32], func=AF.Copy)
        S.op("act", "activation", out=W["E1"][:], in_=W["cum"][:, 0:16], func=AF.Exp)
        S.op("act", "activation", out=W["dtot"][:], in_=W["cum"][:, 16:32], func=AF.Exp)
        S.op("dve", "tensor_tensor", out=W["wgt"][:], in0=W["cum"][:, 16:32], in1=W["cum"][:, 0:16], op=ALU.subtract)
        S.op("act", "activation", out=W["wgt"][:], in_=W["wgt"][:], func=AF.Exp)
        S.op("dve", "tensor_tensor", out=W["wgt"][:], in0=W["wgt"][:], in1=dt_c[:], op=ALU.mult)

    def decay_bufs(P):
        return dict(dta=P.sb("dta", [128, 16]), cum=P.sb("cum", [128, 32]), E1=P.sb("E1", [128, 16]),
                    dtot=P.sb("dtot", [128, 16]), wgt=P.sb("wgt", [128, 16]))

    def state_update(Hs, tok_, W, d, xw, S_ps, Hbf, e2="dve"):
        S.op(e2, "tensor_tensor", out=xw[:].rearrange("p (h q) -> p h q", h=8), in0=tok_[:, 0:512].rearrange("p (h q) -> p h q", h=8),
             in1=W["wgt"][:, d * 8:(d + 1) * 8].unsqueeze(2).to_broadcast([128, 8, 64]), op=ALU.mult)
        for g in range(2):
            S.op("pe", "matmul", out=S_ps[:, g * 256:(g + 1) * 256], lhsT=tok_[:, 512 + g * 128:512 + (g + 1) * 128],
                 rhs=xw[:, g * 256:(g + 1) * 256], start=True, stop=True)
        S.op(e2, "tensor_tensor", out=Hs[:].rearrange("p (h q) -> p h q", h=8), in0=Hs[:].rearrange("p (h q) -> p h q", h=8),
             in1=W["dtot"][:, d * 8:(d + 1) * 8].unsqueeze(2).to_broadcast([128, 8, 64]), op=ALU.mult)
        S.op("dve", "tensor_tensor", out=Hs[:], in0=Hs[:], in1=S_ps[:], op=ALU.add)
        if Hbf is not None:
            S.op("act", "activation", out=Hbf[:], in_=Hs[:], func=AF.Copy)

    def phase_state_sweep(l, sname, d, store, acc_decay=False):
        st = streams[sname]
        T = st["T"]
        nch = T // 128
        P = Pool()
        a_bc = ssd_consts(P, l)
        W = decay_bufs(P)
        cum_ps = P.ps("cum_ps", [128, 32])
        S_ps = P.ps("S_ps", [128, 512])
        toks = [P.sb("tok", [128, 768], BF16) for _ in range(2)]
        dts = [P.sb("dtc", [128, 16]) for _ in range(2)]
        xw = P.sb("xw", [128, 512], BF16)
        Hbf = [P.sb("Hbf", [128, 512], BF16) for _ in range(2)]
        Hs = Hb if d == 1 else Hf
        order = range(nch - 1, -1, -1) if d == 1 else range(nch)
        for i, c in enumerate(order):
            tk, dtc, hb = toks[i % 2], dts[i % 2], Hbf[i % 2]
            S.dma("sp", tk[:], st["xbtok"][c * 128:(c + 1) * 128, :])
            S.dma("sp", dtc[:], st["dt"][c * 128:(c + 1) * 128, :])
            if store:
                S.op("act", "activation", out=hb[:], in_=Hs[:], func=AF.Copy)
                S.dma("pool", st["Hbd"][c], hb[:])
            chunk_decays(P, W, dtc, a_bc, cum_ps)
            if acc_decay:
                S.op("dve", "tensor_tensor", out=Dacc[:], in0=Dacc[:], in1=W["dtot"][:], op=ALU.mult)
            state_update(Hs, tk, W, d, xw, S_ps, None)
        P.close()

    def phase_exchange1():
        st = streams["lat"]
        P = Pool()
        pk = P.sb("pk", [128, W1], BF16)
        for c in (2, 3):
            S.dma("sp", pk[:, (c - 2) * 960:(c - 1) * 960], st["vT"][c * 128:(c + 1) * 128, 0:960])
            S.dma("sp", pk[:, 1920 + (c - 2) * 960:1920 + (c - 1) * 960], st["vT"][c * 128:(c + 1) * 128, L - 960:L])
        xv = pk[:, 3840:3872].rearrange("p (c t) -> p c t", t=4)
        S.dma("sp", xv[:, :, 0:2], st["xbcT"][:, 0:2].rearrange("(c p) t -> p c t", p=128))
        S.dma("sp", xv[:, :, 2:4], st["xbcT"][:, L - 2:L].rearrange("(c p) t -> p c t", p=128))
        S.dma("pool", send1, pk[:])
        S.collective(send1, recv1, ncores)
        accL = P.sb("accL", [128, W1], BF16)
        accR = P.sb("accR", [128, W1], BF16)
        rj = [P.sb("rj", [128, W1], BF16) for _ in range(2)]
        for j in range(ncores):
            r_ = rj[j % 2]
            S.dma("sp", r_[:], recv1[j * 128:(j + 1) * 128, :])
            if j == 0:
                S.op("dve", "tensor_scalar", out=accL[:], in0=r_[:], scalar1=msk[:, 0:1], scalar2=None, op0=ALU.mult)
                S.op("dve", "tensor_scalar", out=accR[:], in0=r_[:], scalar1=msk[:, 8:9], scalar2=None, op0=ALU.mult)
            else:
                S.op("dve", "scalar_tensor_tensor", out=accL[:], in0=r_[:], scalar=msk[:, j:j + 1], in1=accL[:], op0=ALU.mult, op1=ALU.add)
                S.op("dve", "scalar_tensor_tensor", out=accR[:], in0=r_[:], scalar=msk[:, 8 + j:9 + j], in1=accR[:], op0=ALU.mult, op1=ALU.add)
        S.dma("pool", haloL, accL[:])
        S.dma("pool", haloR, accR[:])
        P.close()

    def phase_exchange2():
        P = Pool()
        pk = P.sb("pk2", [128, W2])
        S.op("dve", "tensor_copy", out=pk[:, 0:512], in_=Hf[:])
        S.op("dve", "tensor_copy", out=pk[:, 512:1024], in_=Hb[:])
        S.op("dve", "tensor_copy", out=pk[:, 1024:1040], in_=Dacc[:])
        S.dma("pool", send2, pk[:])
        S.collective(send2, recv2, ncores)
        rj = [P.sb("rj2", [128, W2]) for _ in range(ncores)]
        for j in range(ncores):
            S.dma("sp", rj[j][:], recv2[j * 128:(j + 1) * 128, :])
        dsel = P.sb("dsel", [128, 8])
        S.op("dve", "tensor_copy", out=Hf[:], in_=hf0[:])
        S.op("dve", "tensor_copy", out=Hb[:], in_=hb0[:])
        h8 = lambda r: r.rearrange("p (h q) -> p h q", h=8)
        for d, Hs, order, mo in ((0, Hf, range(ncores), 16), (1, Hb, range(ncores - 1, -1, -1), 24)):
            for j in order:
                r_ = rj[j]
                S.op("dve", "tensor_scalar", out=dsel[:], in0=r_[:, 1024 + d * 8:1032 + d * 8], scalar1=msk[:, mo + j:mo + j + 1],
                     scalar2=omsk[:, mo + j:mo + j + 1], op0=ALU.mult, op1=ALU.add)
                S.op("dve", "tensor_tensor", out=h8(Hs[:]), in0=h8(Hs[:]), in1=dsel[:].unsqueeze(2).to_broadcast([128, 8, 64]), op=ALU.mult)
                S.op("dve", "scalar_tensor_tensor", out=Hs[:], in0=r_[:, d * 512:(d + 1) * 512], scalar=msk[:, mo + j:mo + j + 1], in1=Hs[:],
                     op0=ALU.mult, op1=ALU.add)
        P.close()

    def phase_p4(l, sname):
        st = streams[sname]
        T = st["T"]
        s_idx = 0 if sname == "lat" else 1
        nch = T // 128
        P = Pool()
        a_bc = ssd_consts(P, l)
        W = decay_bufs(P)
        dsk = load_vec_bcast(P, d_skip[l:l + 1, :], 8, "dsk")
        gs = load_vec_bcast(P, ssd_g[l:l + 1, :], 512, "gs")
        GT1 = load_mod_bcast(P, l, s_idx, 2, "GT1")
        wo = P.sb("wo", [128, 8, D], BF16)
        wst = [P.sb("wst", [128, 8, 256]) for _ in range(2)]
        for i in range(4):
            s_ = wst[i % 2]
            S.dma("sp", s_[:], w_out[l, :, i * 256:(i + 1) * 256].rearrange("(c p) n -> p c n", p=128))
            S.op("dve", "tensor_copy", out=wo[:, :, i * 256:(i + 1) * 256], in_=s_[:])
        toks = [P.sb("tok", [128, 768], BF16) for _ in range(3)]
        bcts = [P.sb("bct", [128, 4, 128], BF16) for _ in range(3)]
        dts = [P.sb("dtc", [128, 16]) for _ in range(3)]
        szs = [P.sb("szc", [128, 512]) for _ in range(3)]
        hbs = [P.sb("hbin", [128, 512], BF16) for _ in range(3)]
        cos = [P.sb("coc", [128, 4, 128], BF16) for _ in range(3)]
        hrs = [P.sb("hres", [128, D]) for _ in range(3)]
        Hfb = P.sb("Hfb", [128, 512], BF16)
        xw = P.sb("xw", [128, 512], BF16)
        A_f = P.sb("A_f", [128, 8, 128])
        A_b = P.sb("A_b", [128, 8, 128])
        E_f = P.sb("E_f", [128, 4, 128])
        E_b = P.sb("E_b", [128, 4, 128])
        MT = P.sb("MT", [128, 8, 128], BF16)
        t1 = P.sb("t1", [128, 512])
        t2 = P.sb("t2", [128, 512])
        t3 = P.sb("t3", [128, 512])
        lndt = P.sb("lndt", [128, 16])
        so = P.sb("so", [128, 512], BF16)
        ssq = P.sb("ssq", [128, 4])
        junk = P.sb("junk", [128, 512])
        ssdT = P.sb("ssdT", [128, 4, 128], BF16)
        P0 = P.ps("P0", [128, 512])
        P1 = P.ps("P1", [128, 512])
        P2 = P.ps("P2", [128, 512])
        P3 = P.ps("P3", [128, 512])
        P4 = P.ps("P4", [128, 512])
        P5 = P.ps("P5", [128, 512])
        P6 = P.ps("P6", [128, 512])
        P7 = P.ps("P7", [128, 512], BF16)
        h8 = lambda r: r.rearrange("p (h q) -> p h q", h=8)
        W2 = [W, decay_bufs(P), decay_bufs(P)]
        MTs = [MT, P.sb("MT2", [128, 8, 128], BF16), P.sb("MT3", [128, 8, 128], BF16)]

        def bufs(c):
            i = c % 3
            return toks[i], bcts[i], dts[i], szs[i], hbs[i], cos[i], hrs[i], W2[i], MTs[i]

        def load(c):
            tk, bct, dtc, szc, hbin, coc, hres, W, MT = bufs(c)
            tsl = slice(c * 128, (c + 1) * 128)
            S.dma("sp", tk[:], st["xbtok"][tsl, :])
            S.dma("sp", dtc[:], st["dt"][tsl, :])
            S.dma("sp", bct[:], st["xcT"][:, tsl].rearrange("(c p) t -> p c t", p=128))
            S.dma("sp", szc[:], st["sz"][tsl, :])
            S.dma("sp", hbin[:], st["Hbd"][c])
            S.dma("sp", coc[:], st["coT"][:, tsl].rearrange("(c p) t -> p c t", p=128))
            S.dma("sp", hres[:], st["h"][tsl, :])

        def seg_mm(c, g):
            tk, bct, dtc, szc, hbin, coc, hres, W, MT = bufs(c)
            for hh in range(4):
                h = g * 4 + hh
                S.op("pe", "matmul", out=P1[:, hh * 128:(hh + 1) * 128], lhsT=A_f[:, h, :], rhs=Umat[:], start=True, stop=False)
                S.op("pe", "matmul", out=P1[:, hh * 128:(hh + 1) * 128], lhsT=ident_b[:], rhs=negU[:], start=False, stop=True)
                S.op("pe", "matmul", out=P2[:, hh * 128:(hh + 1) * 128], lhsT=A_b[:, h, :], rhs=Vmat[:], start=True, stop=False)
                S.op("pe", "matmul", out=P2[:, hh * 128:(hh + 1) * 128], lhsT=ident_b[:], rhs=negV[:], start=False, stop=True)

        def seg_exp(c, g):
            for hh in range(4):
                h = g * 4 + hh
                S.op("act", "activation", out=E_f[:, hh, :], in_=P1[:, hh * 128:(hh + 1) * 128], func=AF.Exp, bias=lndt[:, h:h + 1], scale=1.0)
                S.op("act", "activation", out=E_b[:, hh, :], in_=P2[:, hh * 128:(hh + 1) * 128], func=AF.Exp, bias=lndt[:, 8 + h:9 + h], scale=1.0)

        def seg_mt(c, g):
            tk, bct, dtc, szc, hbin, coc, hres, W, MT = bufs(c)
            S.op("dve", "tensor_tensor", out=E_f[:], in0=E_f[:], in1=E_b[:], op=ALU.add)
            S.op("dve", "tensor_tensor", out=MT[:, g * 4:(g + 1) * 4, :], in0=E_f[:],
                 in1=P3[:, g * 128:(g + 1) * 128].unsqueeze(1).to_broadcast([128, 4, 128]), op=ALU.mult)

        def F0(c):
            tk, bct, dtc, szc, hbin, coc, hres, W, MT = bufs(c)
            chunk_decays(P, W, dtc, a_bc, P0)
            S.op("act", "activation", out=lndt[:], in_=dtc[:], func=AF.Ln)
            for g in range(2):
                S.op("pe", "matmul", out=P3[:, g * 128:(g + 1) * 128], lhsT=bct[:, g, :], rhs=bct[:, 2 + g, :], start=True, stop=True)

        def F1(c):
            tk, bct, dtc, szc, hbin, coc, hres, W, MT = bufs(c)
            S.op("dve", "tensor_tensor", out=A_f[:], in0=mS[:].unsqueeze(1).to_broadcast([128, 8, 128]),
                 in1=W["dta"][:, 0:8].unsqueeze(2).to_broadcast([128, 8, 128]), op=ALU.mult)
            S.op("dve", "tensor_tensor", out=A_b[:], in0=mSp[:].unsqueeze(1).to_broadcast([128, 8, 128]),
                 in1=W["dta"][:, 8:16].unsqueeze(2).to_broadcast([128, 8, 128]), op=ALU.mult)

        def F2(c):
            seg_mm(c, 0)

        def F3(c):
            seg_exp(c, 0)
            seg_mm(c, 1)

        def F4(c):
            seg_mt(c, 0)
            seg_exp(c, 1)

        def F5(c):
            seg_mt(c, 1)

        def B0(c):
            tk, bct, dtc, szc, hbin, coc, hres, W, MT = bufs(c)
            S.op("act", "activation", out=Hfb[:], in_=Hf[:], func=AF.Copy)
            for h in range(8):
                S.op("pe", "matmul", out=P4[:, h * 64:(h + 1) * 64], lhsT=MT[:, h, :], rhs=tk[:, h * 64:(h + 1) * 64], start=True, stop=True)
            for g in range(2):
                S.op("pe", "matmul", out=P5[:, g * 256:(g + 1) * 256], lhsT=bct[:, 2 + g, :], rhs=Hfb[:, g * 256:(g + 1) * 256], start=True, stop=True)
                S.op("pe", "matmul", out=P6[:, g * 256:(g + 1) * 256], lhsT=bct[:, 2 + g, :], rhs=hbin[:, g * 256:(g + 1) * 256], start=True, stop=True)
            S.op("dve", "tensor_tensor", out=h8(t3[:]), in0=h8(tk[:, 0:512]), in1=dsk[:].unsqueeze(2).to_broadcast([128, 8, 64]), op=ALU.mult)
            S.op("dve", "tensor_tensor", out=h8(t1[:]), in0=h8(P5[:]), in1=W["E1"][:, 0:8].unsqueeze(2).to_broadcast([128, 8, 64]), op=ALU.mult)
            S.op("dve", "tensor_tensor", out=h8(t2[:]), in0=h8(P6[:]), in1=W["E1"][:, 8:16].unsqueeze(2).to_broadcast([128, 8, 64]), op=ALU.mult)

        def B1(c):
            tk, bct, dtc, szc, hbin, coc, hres, W, MT = bufs(c)
            tsl = slice(c * 128, (c + 1) * 128)
            S.op("dve", "tensor_tensor", out=t1[:], in0=t1[:], in1=t2[:], op=ALU.add)
            S.op("dve", "tensor_tensor", out=t1[:], in0=t1[:], in1=P4[:], op=ALU.add)
            S.op("dve", "tensor_tensor", out=t1[:], in0=t1[:], in1=t3[:], op=ALU.add)
            if "dbg_y" in dbg:
                S.dma("pool", dbg_y[sname][tsl, :], t1[:])

        def B2(c):
            tk, bct, dtc, szc, hbin, coc, hres, W, MT = bufs(c)
            state_update(Hf, tk, W, 0, xw, P5, None, e2="dve")
            S.op("dve", "tensor_tensor", out=t1[:], in0=t1[:], in1=szc[:], op=ALU.mult)

        def B3(c):
            S.op("act", "activation", out=junk[:], in_=t1[:], func=AF.Square, accum_out=ssq[:, 0:1])
            rstd_from_ssq(P, ssq, 1, 1.0 / 512, RMS_EPS)
            S.op("dve", "scalar_tensor_tensor", out=so[:], in0=t1[:], scalar=ssq[:, 0:1], in1=gs[:], op0=ALU.mult, op1=ALU.mult)
            for k4 in range(4):
                S.op("pe", "transpose", out=P7[:, k4 * 128:(k4 + 1) * 128], in_=so[:, k4 * 128:(k4 + 1) * 128], identity=ident_b[:])
            S.op("act", "activation", out=ssdT[:].rearrange("p c t -> p (c t)"), in_=P7[:], func=AF.Copy)

        def outproj(c, half, PO, tq):
            tk, bct, dtc, szc, hbin, coc, hres, W, MT = bufs(c)
            for kc in range(8):
                lh = coc[:, kc, :] if kc < 4 else ssdT[:, kc - 4, :]
                S.op("pe", "matmul", _ni=True, out=PO[:], lhsT=lh, rhs=wo[:, kc, half * 512:(half + 1) * 512], start=(kc == 0), stop=(kc == 7))
            S.op("dve", "tensor_tensor", out=tq[:], in0=PO[:], in1=GT1[:, half * 512:(half + 1) * 512], op=ALU.mult)
            S.op("dve", "tensor_tensor", out=hres[:, half * 512:(half + 1) * 512], in0=hres[:, half * 512:(half + 1) * 512], in1=tq[:], op=ALU.add)

        def B4(c):
            outproj(c, 0, P6, t2)

        def B5(c):
            tk, bct, dtc, szc, hbin, coc, hres, W, MT = bufs(c)
            tsl = slice(c * 128, (c + 1) * 128)
            outproj(c, 1, P4, t3)
            S.dma("pool", st["h"][tsl, :], hres[:])

        FS = [F0, F1, F2, F3, F4, F5]
        BS = [B0, B1, B2, B3, B4, B5]
        load(0)
        if nch > 1:
            load(1)
        for f in FS:
            f(0)
        for c in range(nch):
            if c + 2 < nch:
                load(c + 2)
            for k in range(6):
                if c + 1 < nch:
                    FS[k](c + 1)
                BS[k](c)
        P.close()

    def phase_p6(l, sname, final):
        st = streams[sname]
        T = st["T"]
        s_idx = 0 if sname == "lat" else 1
        TB = min(1024, T)
        TW = min(512, TB)
        nsubB = TB // 128
        nsubW = TW // 128
        nblk = T // TB
        P = Pool()
        G2 = load_mod_bcast(P, l, s_idx, 4, "G2")
        S2 = load_mod_bcast(P, l, s_idx, 3, "S2")
        GT2 = load_mod_bcast(P, l, s_idx, 5, "GT2")
        gf = load_vec_bcast(P, g_ffn[l:l + 1, :], D, "gf")
        S.op("dve", "scalar_tensor_tensor", out=G2[:], in0=G2[:], scalar=1.0, in1=gf[:], op0=ALU.add, op1=ALU.mult)
        if final:
            S.dma("sp", gf[:], g_final[0:1, :].partition_broadcast(128))
        brb = load_vec_bcast(P, b_r[l:l + 1, :], 20, "brb")
        wr = P.sb("wr", [128, 8, 20])
        S.dma("sp", wr[:], w_r[l].rearrange("(c p) n -> p c n", p=128))
        hts = [P.sb("ht", [128, D]) for _ in range(2)]
        hcs = [P.sb("hc", [128, D]) for _ in range(2)]
        junkA = P.sb("junkA", [128, D])
        junkC = P.sb("junkC", [128, D])
        ssqA = P.sb("ssqA", [128, 4])
        ssqC = P.sb("ssqC", [128, 4])
        h2 = P.sb("h2", [128, D])
        h2Tf = P.sb("h2Tf", [128, 8, 128])
        h2Ts = [P.sb("h2T", [128, 8, TB], BF16) for _ in range(2)]
        maccs = [[P.sb("macc%d" % hh, [128, nsubB, 512]) for hh in range(2)] for _ in range(2)]
        tmps = [P.sb("tmpm", [128, 512]) for _ in range(2)]
        combss = [P.sb("combs", [128, nsubB, 16]) for _ in range(2)]
        lgg = P.sb("lgg", [128, nsubB, 4])
        lge = P.sb("lge", [128, nsubB, 16])
        r_gmax = P.sb("r_gmax", [128, nsubB])
        r_gmask = P.sb("r_gmask", [128, nsubB, 4])
        r_gexp = P.sb("r_gexp", [128, nsubB, 4])
        r_gsum = P.sb("r_gsum", [128, nsubB])
        r_elm = P.sb("r_elm", [128, nsubB, 16])
        r_m1 = P.sb("r_m1", [128, nsubB])
        r_m2 = P.sb("r_m2", [128, nsubB])
        r_mask1 = P.sb("r_mask1", [128, nsubB, 16])
        r_mask2 = P.sb("r_mask2", [128, nsubB, 16])
        wgus = [P.sb("wgu", [128, 8, 2 * FF], BF16) for _ in range(2)]
        wds = [P.sb("wd", [128, 2, D], BF16) for _ in range(2)]
        sgs = [P.sb("sg", [128, 512]) for _ in range(2)]
        hTs = [P.sb("hT", [128, 2, 512], BF16) for _ in range(2)]
        Q = [P.ps("Q%d" % i, [128, 512]) for i in range(8)]
        pgs, pus, pos = [Q[0], Q[1]], [Q[3], Q[4]], [Q[5], Q[6]]
        qa, qb = Q[2], Q[7]

        def A0(bi, j):
            tsl = slice(bi * TB + j * 128, bi * TB + (j + 1) * 128)
            S.dma("sp", hts[j % 2][:], st["h"][tsl, :])

        def A1(bi, j):
            h_ = hts[j % 2]
            S.op("act", "activation", out=junkA[:], in_=h_[:], func=AF.Square, accum_out=ssqA[:, 0:1])
            rstd_from_ssq(P, ssqA, 1, 1.0 / D, RMS_EPS)
            S.op("dve", "scalar_tensor_tensor", out=h2[:], in0=h_[:], scalar=ssqA[:, 0:1], in1=G2[:], op0=ALU.mult, op1=ALU.mult)
            S.op("dve", "tensor_tensor", out=h2[:], in0=h2[:], in1=S2[:], op=ALU.add)

        def A2(bi, j):
            for hf_, q_ in ((0, qa), (1, qb)):
                for kc in range(4):
                    kk = hf_ * 4 + kc
                    S.op("pe", "transpose", out=q_[:, kc * 128:(kc + 1) * 128], in_=h2[:, kk * 128:(kk + 1) * 128], identity=ident_f[:])
            for hf_, q_ in ((0, qa), (1, qb)):
                S.op("act", "activation", out=h2Tf[:, hf_ * 4:(hf_ + 1) * 4, :].rearrange("p c t -> p (c t)"), in_=q_[:], func=AF.Copy)

        def A3(bi, j):
            h2T = h2Ts[bi % 2]
            S.op("dve", "tensor_copy", out=h2T[:, :, j * 128:(j + 1) * 128], in_=h2Tf[:])
            for kc in range(8):
                S.op("pe", "matmul", _ni=True, out=qa[:, 0:20], lhsT=h2Tf[:, kc, :], rhs=wr[:, kc, :], start=(kc == 0), stop=(kc == 7))
            S.op("dve", "tensor_tensor", out=lgg[:, j, :], in0=qa[:, 0:4], in1=brb[:, 0:4], op=ALU.add)
            S.op("dve", "tensor_tensor", out=lge[:, j, :], in0=qa[:, 4:20], in1=brb[:, 4:20], op=ALU.add)

        def A_stages(bi):
            out = [[lambda: A0(bi, 0)]]
            for j in range(nsubB):
                nxt = [lambda j=j: A0(bi, j + 1)] if j + 1 < nsubB else []
                out.append([lambda j=j: A1(bi, j)] + nxt)
                out.append([lambda j=j: A2(bi, j)])
                out.append([lambda j=j: A3(bi, j)])
            out.append([lambda: router(bi)])
            return out

        def router(bi):
            combs = combss[bi % 2]
            V_ = lambda name, **kw: S.op("dve", name, **kw)
            bc = lambda r, n: r.unsqueeze(2).to_broadcast([128, r.ap.shape[1], n])
            V_("tensor_reduce", out=r_gmax[:], in_=lgg[:], axis=AX.X, op=ALU.max)
            V_("tensor_tensor", out=r_gmask[:], in0=lgg[:], in1=bc(r_gmax[:], 4), op=ALU.is_ge)
            V_("tensor_tensor", out=r_gexp[:], in0=lgg[:], in1=bc(r_gmax[:], 4), op=ALU.subtract)
            S.op("act", "activation", out=r_gexp[:], in_=r_gexp[:], func=AF.Exp)
            V_("tensor_reduce", out=r_gsum[:], in_=r_gexp[:], axis=AX.X, op=ALU.add)
            V_("reciprocal", out=r_gsum[:], in_=r_gsum[:])
            V_("tensor_scalar", out=r_gmask[:], in0=r_gmask[:], scalar1=-1.0, scalar2=-NEG, op0=ALU.add, op1=ALU.mult)
            pen = r_gmask[:].rearrange("p j g -> p (j g)")
            V_("tensor_tensor", out=r_elm[:].rearrange("p j (g e) -> p (j g) e", g=4), in0=lge[:].rearrange("p j (g e) -> p (j g) e", g=4),
               in1=bc(pen, 4), op=ALU.add)
            V_("tensor_reduce", out=r_m1[:], in_=r_elm[:], axis=AX.X, op=ALU.max)
            V_("tensor_tensor", out=r_mask1[:], in0=r_elm[:], in1=bc(r_m1[:], 16), op=ALU.is_ge)
            V_("scalar_tensor_tensor", out=r_elm[:], in0=r_mask1[:], scalar=NEG, in1=r_elm[:], op0=ALU.mult, op1=ALU.add)
            V_("tensor_reduce", out=r_m2[:], in_=r_elm[:], axis=AX.X, op=ALU.max)
            V_("tensor_tensor", out=r_mask2[:], in0=r_elm[:], in1=bc(r_m2[:], 16), op=ALU.is_ge)
            V_("tensor_tensor", out=r_m2[:], in0=r_m2[:], in1=r_m1[:], op=ALU.subtract)
            S.op("act", "activation", out=r_m2[:], in_=r_m2[:], func=AF.Exp)
            V_("tensor_scalar", out=r_m1[:], in0=r_m2[:], scalar1=1.0, scalar2=None, op0=ALU.add)
            V_("reciprocal", out=r_m1[:], in_=r_m1[:])
            V_("tensor_tensor", out=r_m1[:], in0=r_m1[:], in1=r_gsum[:], op=ALU.mult)
            V_("tensor_tensor", out=r_m2[:], in0=r_m1[:], in1=r_m2[:], op=ALU.mult)
            V_("tensor_tensor", out=r_mask1[:], in0=r_mask1[:], in1=bc(r_m1[:], 16), op=ALU.mult)
            V_("tensor_tensor", out=r_mask2[:], in0=r_mask2[:], in1=bc(r_m2[:], 16), op=ALU.mult)
            V_("tensor_tensor", out=combs[:], in0=r_mask1[:], in1=r_mask2[:], op=ALU.add)

        def C0(bi, j):
            tsl = slice(bi * TB + j * 128, bi * TB + (j + 1) * 128)
            S.dma("sp", hcs[j % 2][:], st["h"][tsl, :])

        def C1(bi, j):
            macc = maccs[bi % 2]
            h_ = hcs[j % 2]
            tsl = slice(bi * TB + j * 128, bi * TB + (j + 1) * 128)
            for hh in range(2):
                hs = slice(hh * 512, (hh + 1) * 512)
                S.op("dve", "tensor_tensor", out=macc[hh][:, j, :], in0=macc[hh][:, j, :], in1=GT2[:, hs], op=ALU.mult)
                S.op("dve", "tensor_tensor", out=h_[:, hs], in0=h_[:, hs], in1=macc[hh][:, j, :], op=ALU.add)
            if not final:
                S.dma("pool", st["h"][tsl, :], h_[:])

        def C2(bi, j):
            h_ = hcs[j % 2]
            S.op("act", "activation", out=junkC[:], in_=h_[:], func=AF.Square, accum_out=ssqC[:, 1:2])
            S.op("act", "activation", out=ssqC[:, 1:2], in_=ssqC[:, 1:2], func=AF.Ln, bias=RMS_EPS, scale=1.0 / D)
            S.op("act", "activation", out=ssqC[:, 1:2], in_=ssqC[:, 1:2], func=AF.Exp, scale=-0.5)

        def C3(bi, j):
            h_ = hcs[j % 2]
            tsl = slice(bi * TB + j * 128, bi * TB + (j + 1) * 128)
            S.op("dve", "scalar_tensor_tensor", out=h_[:], in0=h_[:], scalar=ssqC[:, 1:2], in1=gf[:], op0=ALU.mult, op1=ALU.mult)
            S.dma("pool", out_d[tsl, :], h_[:])

        def C_stages(bi):
            out = [[lambda: C0(bi, 0)]]
            for j in range(nsubB):
                nxt = [lambda j=j: C0(bi, j + 1)] if j + 1 < nsubB else []
                if final:
                    out.append([lambda j=j: C1(bi, j)])
                    out.append([lambda j=j: C2(bi, j)])
                    out.append([lambda j=j: C3(bi, j)] + nxt)
                else:
                    out.append([lambda j=j: C1(bi, j)] + nxt)
            return out

        ipc = [0]
        itc = [0]
        tpc = [0]

        def emit_down(bi, item, idx, subs):
            e, tw0 = item
            hT = hTs[idx % 2]
            wd = wds[e % 2]
            macc, combs = maccs[bi % 2], combss[bi % 2]
            for js in subs:
                j = tw0 // 128 + js
                for half in range(2):
                    p_ = pos[ipc[0] % 2]
                    ipc[0] += 1
                    for fc in range(2):
                        S.op("pe", "matmul", _ni=True, out=p_[:], lhsT=hT[:, fc, js * 128:(js + 1) * 128], rhs=wd[:, fc, half * 512:(half + 1) * 512], start=(fc == 0), stop=(fc == 1))
                    mslice = macc[half][:, j, :]
                    if half == 0:
                        if e == 0:
                            S.op("dve", "tensor_scalar", out=mslice, in0=p_[:], scalar1=combs[:, j, e:e + 1], scalar2=None, op0=ALU.mult)
                        else:
                            S.op("dve", "scalar_tensor_tensor", out=mslice, in0=p_[:], scalar=combs[:, j, e:e + 1], in1=mslice, op0=ALU.mult, op1=ALU.add)
                    else:
                        if e == 0:
                            S.op("act", "activation", out=mslice, in_=p_[:], func=AF.Copy, scale=combs[:, j, e:e + 1])
                        else:
                            tmp = tmps[tpc[0] % 2]
                            tpc[0] += 1
                            S.op("act", "activation", out=tmp[:], in_=p_[:], func=AF.Copy, scale=combs[:, j, e:e + 1])
                            S.op("pool", "tensor_tensor", out=mslice, in0=mslice, in1=tmp[:], op=ALU.add)

        def B_block(bi, extras):
            h2T = h2Ts[bi % 2]
            items = [(e, tw0) for e in range(NE) for tw0 in range(0, TB, TW)]
            n_it = len(items)
            slots = {}
            for k, fn in enumerate(extras):
                slots.setdefault(min(n_it - 1, (k * n_it) // max(1, len(extras))), []).append(fn)
            for idx, (e, tw0) in enumerate(items):
                wgu = wgus[e % 2]
                if tw0 == 0:
                    S.dma("sp", wgu[:], wgu_bf[e])
                    S.dma("sp", wds[e % 2][:], wd_bf[e])
                hT = hTs[idx % 2]
                for fc in range(2):
                    pg, pu, sg = pgs[itc[0] % 2], pus[itc[0] % 2], sgs[itc[0] % 2]
                    itc[0] += 1
                    for kc in range(8):
                        S.op("pe", "matmul", _ni=True, out=pg[:, 0:TW], lhsT=wgu[:, kc, fc * 128:(fc + 1) * 128], rhs=h2T[:, kc, tw0:tw0 + TW], start=(kc == 0), stop=(kc == 7))
                    for kc in range(8):
                        S.op("pe", "matmul", _ni=True, out=pu[:, 0:TW], lhsT=wgu[:, kc, FF + fc * 128:FF + (fc + 1) * 128], rhs=h2T[:, kc, tw0:tw0 + TW], start=(kc == 0), stop=(kc == 7))
                    S.op("act", "activation", out=sg[:, 0:TW], in_=pg[:, 0:TW], func=AF.Silu)
                    S.op("dve", "tensor_tensor", out=hT[:, fc, 0:TW], in0=pu[:, 0:TW], in1=sg[:, 0:TW], op=ALU.mult)
                    if idx > 0:
                        half_n = (nsubW + 1) // 2
                        subs = range(0, half_n) if fc == 0 else range(half_n, nsubW)
                        emit_down(bi, items[idx - 1], idx - 1, subs)
                for grp in slots.get(idx, []):
                    for fn in grp:
                        fn()
            emit_down(bi, items[-1], n_it - 1, range(nsubW))

        for grp in A_stages(0):
            for fn in grp:
                fn()
        for bi in range(nblk):
            extras = []
            if bi + 1 < nblk:
                extras += A_stages(bi + 1)
            if bi > 0:
                extras += C_stages(bi - 1)
            B_block(bi, extras)
        for grp in C_stages(nblk - 1):
            for fn in grp:
                fn()
        P.close()

    dbg_y = {}
    if "dbg_y" in dbg:
        dbg_y = {"lat": nc.dram_tensor("dbg_y_lat", [L, 512], F32, kind="ExternalOutput").ap(),
                 "ctx": nc.dram_tensor("dbg_y_ctx", [CTX, 512], F32, kind="ExternalOutput").ap()}

    def zero_states():
        memset("dve", Hf[:], 0.0)
        memset("dve", Hb[:], 0.0)

    class Stop(Exception):
        pass

    S.marks = []

    def mark(name):
        S.marks.append((name, dict(S.cnt)))
        if stop_after == name:
            raise Stop()

    try:
        phase_copy_in()
        mark("copy")
        for l in range(depth):
            last = l == depth - 1
            phase_ada(l)
            mark("ada%d" % l)
            phase_wprep(l)
            mark("wprep%d" % l)
            phase_p1(l, "ctx")
            mark("p1c%d" % l)
            phase_p2b(l, "ctx")
            mark("p2bc%d" % l)
            zero_states()
            if last:
                phase_state_sweep(l, "ctx", 1, False)
                phase_state_sweep(l, "ctx", 0, False)
            else:
                phase_p2(l, "ctx")
                phase_state_sweep(l, "ctx", 1, True)
                phase_p4(l, "ctx")
            mark("ctxmix%d" % l)
            phase_p1(l, "lat")
            mark("p1%d" % l)
            if multi:
                phase_exchange1()
                mark("ex1_%d" % l)
            phase_p2(l, "lat")
            mark("p2%d" % l)
            phase_p2b(l, "lat")
            mark("p2b%d" % l)
            if multi:
                S.op("dve", "tensor_copy", out=hf0[:], in_=Hf[:])
                S.op("dve", "tensor_copy", out=hb0[:], in_=Hb[:])
                zero_states()
                memset("dve", Dacc[:], 1.0)
                phase_state_sweep(l, "lat", 0, False, acc_decay=True)
                phase_state_sweep(l, "lat", 1, False)
                phase_exchange2()
                mark("ex2_%d" % l)
            phase_state_sweep(l, "lat", 1, True)
            mark("p3%d" % l)
            phase_p4(l, "lat")
            mark("p4%d" % l)
            phase_p6(l, "lat", last)
            mark("p6%d" % l)
            if not last:
                phase_p6(l, "ctx", False)
            mark("p6c%d" % l)
    except Stop:
        pass
    CP.close()
    return nc, S


def host_weights(inputs, depth=2):
    f = lambda a: np.ascontiguousarray(np.asarray(a, dtype=np.float32))
    m = {}
    for k in ("w_ada", "b_ada", "g_mix", "g_ffn", "w_in", "ssd_norm_g", "w_out", "w_gate", "w_up", "w_down", "d_skip"):
        m[k] = f(inputs[k])
    cw = np.asarray(inputs["conv_w"])
    m["conv_wT"] = f(cw.transpose(0, 2, 1).reshape(depth, 4, 128, 31).transpose(0, 2, 1, 3))
    pc = np.stack([np.asarray(inputs["conv_b"]), np.asarray(inputs["conv_ln_g"]), np.asarray(inputs["conv_ln_b"])], axis=1)
    m["conv_pc"] = f(pc.reshape(depth, 3, 4, 128).transpose(0, 3, 1, 2))
    sw = np.asarray(inputs["ssd_conv_w"])
    m["sconv_wT"] = f(sw.transpose(0, 2, 1).reshape(depth, 8, 128, 5).transpose(0, 2, 1, 3))
    m["sconv_b"] = f(np.asarray(inputs["ssd_conv_b"]).reshape(depth, 8, 128).transpose(0, 2, 1))
    m["dt_bias"] = f(np.asarray(inputs["dt_bias"]).reshape(depth, 16))
    m["a_log"] = f(np.asarray(inputs["a_log"]).reshape(depth, 16))
    m["w_r"] = f(np.concatenate([inputs["w_router_group"], inputs["w_router_expert"]], axis=2))
    m["b_r"] = f(np.concatenate([inputs["b_router_group"], inputs["b_router_expert"]], axis=1))
    m["g_final"] = f(np.asarray(inputs["g_final"]).reshape(1, D))
    return m


def host_core_map(inputs, wmap, k, ncores, nq):
    f = lambda a: np.ascontiguousarray(np.asarray(a, dtype=np.float32))
    b, q = k // nq, k % nq
    Ltot = np.asarray(inputs["x"]).shape[1]
    LQ = Ltot // nq
    m = dict(wmap)
    m["x"] = f(inputs["x"][b, q * LQ:(q + 1) * LQ])
    m["ctx"] = f(inputs["ctx"][b])
    m["cvT"] = f(np.stack([inputs["c"][b], inputs["c_ctx"]], axis=1))
    msk = np.zeros((1, 32), np.float32)
    for j in range(ncores):
        same = (j // nq) == b
        if same and j == k - 1:
            msk[0, j] = 1.0
        if same and j == k + 1:
            msk[0, 8 + j] = 1.0
        if same and j < k:
            msk[0, 16 + j] = 1.0
        if same and j > k:
            msk[0, 24 + j] = 1.0
    m["msk"] = msk
    return m


_CACHE = {}
NQ = 1
NCORES = 2


def kernel(**inputs):
    x = np.asarray(inputs["x"])
    B, L, _ = x.shape
    CTX = np.asarray(inputs["ctx"]).shape[1]
    depth = np.asarray(inputs["w_in"]).shape[0]
    nwork = B * NQ
    LQ = L // NQ
    key = (LQ, CTX, depth, nwork if NQ > 1 else 1)
    if key not in _CACHE:
        _CACHE[key] = build(LQ, CTX, depth, ncores=key[3])[0]
    nc = _CACHE[key]
    wmap = host_weights(inputs, depth)
    work = [host_core_map(inputs, wmap, k, nwork, NQ) for k in range(nwork)]
    in_maps = [work[k % nwork] for k in range(NCORES)]
    res = run_bass_kernel_spmd(nc, in_maps, core_ids=list(range(NCORES)))
    out = np.empty((B, L, D), np.float32)
    for k in range(nwork):
        b, q = k // NQ, k % NQ
        out[b, q * LQ:(q + 1) * LQ] = np.asarray(res.results[k]["out"])
    return out
```

```python
import numpy as np
import concourse.bass as bass
import concourse.mybir as mybir
from concourse.bass_utils import run_bass_kernel_spmd

F32 = mybir.dt.float32
BF16 = mybir.dt.bfloat16
AF = mybir.ActivationFunctionType
ALU = mybir.AluOpType
AX = mybir.AxisListType

D = 1024
DIN = 2576
NE = 16
FF = 256
RMS_EPS = 1e-6
LN_EPS = 1e-5
NEG = -1.0e30


class Ref:
    __slots__ = ("ap", "buf")

    def __init__(self, ap, buf):
        self.ap = ap
        self.buf = buf

    def __getitem__(self, k):
        return Ref(self.ap[k], self.buf)

    def rearrange(self, s, **kw):
        return Ref(self.ap.rearrange(s, **kw), self.buf)

    def unsqueeze(self, a):
        return Ref(self.ap.unsqueeze(a), self.buf)

    def to_broadcast(self, shape):
        return Ref(self.ap.to_broadcast(shape), self.buf)


class Buf:
    def __init__(self, handle, name):
        self.t = handle
        self.name = name
        self.last_w = None
        self.readers = {}
        self.dsem = None

    def __getitem__(self, k):
        return Ref(self.t[k], self)


class Sched:
    ENG = ("pe", "act", "dve", "pool", "sp")

    def __init__(self, nc):
        self.nc = nc
        self.eng = {"pe": nc.tensor, "act": nc.scalar, "dve": nc.vector, "pool": nc.gpsimd, "sp": nc.sync}
        self.sems = {}
        self.cnt = {}
        for e in self.ENG:
            self.sems[e] = nc.alloc_semaphore("s_" + e)
            self.cnt[e] = 0
        self.known = {e: {} for e in self.ENG}
        self.free_dsems = []
        self.n_dsem = 0
        self.ninst = 0

    def _get_dsem(self, buf):
        if buf.dsem is None:
            if self.free_dsems:
                buf.dsem = self.free_dsems.pop()
            else:
                key = "d%d" % self.n_dsem
                self.n_dsem += 1
                self.sems[key] = self.nc.alloc_semaphore("s_" + key)
                self.cnt[key] = 0
                buf.dsem = key
        return buf.dsem

    def release(self, bufs):
        for b in bufs:
            if b.dsem is not None:
                self.free_dsems.append(b.dsem)
                b.dsem = None

    def _wait(self, e, need):
        eng = self.eng[e]
        kn = self.known[e]
        for k, v in need.items():
            if kn.get(k, 0) < v:
                eng.wait_ge(self.sems[k], v)
                kn[k] = v
                self.ninst += 1

    def _deps(self, e, reads, writes):
        need = {}

        def add(ev):
            if ev is None:
                return
            k, v = ev
            if e == "pe" and k == "pe":
                return
            if need.get(k, 0) < v:
                need[k] = v
        for b in reads:
            add(b.last_w)
        for b in writes:
            add(b.last_w)
            for k, v in b.readers.items():
                add((k, v))
        return need

    def op(self, e, name, _ni=False, **kw):
        reads, writes, args = [], [], {}
        for k, v in kw.items():
            if isinstance(v, Ref):
                (writes if k in ("out", "accum_out") else reads).append(v.buf)
                args[k] = v.ap
            else:
                args[k] = v
        self._wait(e, self._deps(e, reads, writes))
        ins = getattr(self.eng[e], name)(**args)
        self.ninst += 1
        if _ni and e == "pe" and name == "matmul" and kw.get("stop") is False:
            ev = (e, self.cnt[e] + 1)
        else:
            ins.then_inc(self.sems[e], 1)
            self.cnt[e] += 1
            ev = (e, self.cnt[e])
        for b in reads:
            if b not in writes:
                b.readers[e] = ev[1]
        for b in writes:
            b.last_w = ev
            b.readers = {}
        return ins

    def dma(self, q, out, in_):
        reads, writes = [], []
        o = out
        i = in_
        if isinstance(out, Ref):
            writes.append(out.buf)
            o = out.ap
        if isinstance(in_, Ref):
            reads.append(in_.buf)
            i = in_.ap
        self._wait(q, self._deps(q, reads, writes))
        tb = (writes + reads)[0]
        key = self._get_dsem(tb)
        self.eng[q].dma_start(out=o, in_=i).then_inc(self.sems[key], 16)
        self.cnt[key] += 16
        self.ninst += 1
        ev = (key, self.cnt[key])
        for b in reads:
            b.readers[key] = ev[1]
        for b in writes:
            b.last_w = ev
            b.readers = {}

    def collective(self, ins_ap, outs_ap, ncores):
        if "cc" not in self.sems:
            self.sems["cc"] = self.nc.alloc_semaphore("s_cc")
            self.cnt["cc"] = 0
        self.barrier()
        ins = self.nc.gpsimd.collective_compute("AllGather", ALU.bypass, replica_groups=[list(range(ncores))],
                                                ins=[ins_ap.opt()], outs=[outs_ap.opt()])
        ins.then_inc(self.sems["cc"], 1)
        self.cnt["cc"] += 1
        self.ninst += 1
        self.barrier()

    def barrier(self):
        for e in self.ENG:
            need = {k: v for k, v in self.cnt.items() if v > 0 and k != e}
            self._wait(e, need)


def build(L, CTX, depth=2, dbg=(), stop_after=None, ncores=1):
    multi = ncores > 1
    nc = bass.Bass("TRN2", target_bir_lowering=False)
    S = Sched(nc)
    ROWS = L // 64
    dbg = set(dbg)

    def din(name, shape, dt=F32):
        return nc.dram_tensor(name, list(shape), dt, kind="ExternalInput").ap()

    def dscr(name, shape, dt=F32):
        kind = "ExternalOutput" if name in dbg else "Internal"
        return nc.dram_tensor(name, list(shape), dt, kind=kind).ap()

    x_in = din("x", [L, D])
    ctx_in = din("ctx", [CTX, D])
    cvT = din("cvT", [D, 2])
    w_ada = din("w_ada", [depth, D, 6 * D])
    b_ada = din("b_ada", [depth, 6 * D])
    g_mix = din("g_mix", [depth, D])
    g_ffn = din("g_ffn", [depth, D])
    w_in = din("w_in", [depth, D, DIN])
    conv_wT = din("conv_wT", [depth, 128, 4, 31])
    conv_pc = din("conv_pc", [depth, 128, 3, 4])
    sconv_wT = din("sconv_wT", [depth, 128, 8, 5])
    sconv_b = din("sconv_b", [depth, 128, 8])
    dt_bias = din("dt_bias", [depth, 16])
    a_log = din("a_log", [depth, 16])
    d_skip = din("d_skip", [depth, 8])
    ssd_g = din("ssd_norm_g", [depth, 512])
    w_out = din("w_out", [depth, D, D])
    w_r = din("w_r", [depth, D, 20])
    b_r = din("b_r", [depth, 20])
    w_gate = din("w_gate", [depth, NE, D, FF])
    w_up = din("w_up", [depth, NE, D, FF])
    w_down = din("w_down", [depth, NE, FF, D])
    g_final = din("g_final", [1, D])
    msk_in = din("msk", [1, 32])
    out_d = nc.dram_tensor("out", [L, D], F32, kind="ExternalOutput").ap()

    modd = dscr("modd", [depth, 2, 6 * D])
    wgu_bf = dscr("wgu_bf", [NE, 128, 8, 2 * FF], BF16)
    wd_bf = dscr("wd_bf", [NE, 128, 2, D], BF16)
    W1 = 3872
    W2 = 1040
    send1 = dscr("send1", [128, W1], BF16)
    recv1 = dscr("recv1", [max(ncores, 1) * 128, W1], BF16)
    haloL = dscr("haloL", [128, W1], BF16)
    haloR = dscr("haloR", [128, W1], BF16)
    send2 = dscr("send2", [128, W2])
    recv2 = dscr("recv2", [max(ncores, 1) * 128, W2])
    streams = {}
    for sname, T in (("lat", L), ("ctx", CTX)):
        streams[sname] = dict(
            T=T,
            h=dscr(sname + "_h", [T, D]),
            vT=dscr(sname + "_vT", [512, T], BF16),
            xbcT=dscr(sname + "_xbcT", [1024, T], BF16),
            sz=dscr(sname + "_sz", [T, 512]),
            dt=dscr(sname + "_dt", [T, 16]),
            coT=dscr(sname + "_coT", [512, T], BF16),
            xcT=dscr(sname + "_xcT", [512, T], BF16),
            xbtok=dscr(sname + "_xbtok", [T, 768], BF16),
            Hbd=dscr(sname + "_Hbd", [T // 128, 128, 512], BF16),
        )

    class Pool:
        uid = [0]

        def __init__(self):
            self.stack = []
            self.bufs = []

        def _nm(self, name):
            Pool.uid[0] += 1
            return "%s_%d" % (name, Pool.uid[0])

        def sb(self, name, shape, dt=F32):
            g = nc.sbuf_tensor(self._nm(name), list(shape), dt)
            h = g.__enter__()
            self.stack.append(g)
            b = Buf(h, name)
            self.bufs.append(b)
            return b

        def ps(self, name, shape, dt=F32):
            g = nc.psum_tensor(self._nm(name), list(shape), dt)
            h = g.__enter__()
            self.stack.append(g)
            b = Buf(h, name)
            self.bufs.append(b)
            return b

        def close(self):
            S.barrier()
            S.release(self.bufs)
            for g in reversed(self.stack):
                g.__exit__(None, None, None)
            self.stack = []
            self.bufs = []

    CP = Pool()
    ident_f = CP.sb("ident_f", [128, 128])
    ident_b = CP.sb("ident_b", [128, 128], BF16)
    ones_f = CP.sb("ones_f", [128, 128])
    Umat = CP.sb("Umat", [128, 128])
    Vmat = CP.sb("Vmat", [128, 128])
    mS = CP.sb("mS", [128, 128])
    mSp = CP.sb("mSp", [128, 128])
    sel = CP.sb("sel", [16, NE, 128])
    Hf = CP.sb("Hf", [128, 512])
    Hb = CP.sb("Hb", [128, 512])
    hf0 = CP.sb("hf0", [128, 512])
    hb0 = CP.sb("hb0", [128, 512])
    Dacc = CP.sb("Dacc", [128, 16])
    msk = CP.sb("msk", [128, 32])
    omsk = CP.sb("omsk", [128, 32])

    def memset(e, ref, val):
        reads, writes = [], [ref.buf]
        S._wait(e, S._deps(e, reads, writes))
        ins = S.eng[e].memset(ref.ap, val)
        ins.then_inc(S.sems[e], 1)
        S.cnt[e] += 1
        S.ninst += 1
        ref.buf.last_w = (e, S.cnt[e])
        ref.buf.readers = {}

    def asel(ref, pattern, cm, cmp, fill, base=0):
        S.op("pool", "affine_select", out=ref, in_=ref, pattern=pattern, compare_op=cmp, fill=fill,
             base=base, channel_multiplier=cm)

    def cast(e, out, in_):
        if e == "act":
            S.op("act", "activation", out=out, in_=in_, func=AF.Copy)
        else:
            S.op(e, "tensor_copy", out=out, in_=in_)

    memset("pool", ident_f[:], 0.0)
    asel(ident_f[:], [[-1, 128]], 1, ALU.not_equal, 1.0)
    S.op("dve", "tensor_copy", out=ident_b[:], in_=ident_f[:])
    memset("pool", ones_f[:], 1.0)
    memset("pool", Umat[:], 1.0)
    asel(Umat[:], [[1, 128]], -1, ALU.is_ge, 0.0)
    memset("pool", Vmat[:], 1.0)
    asel(Vmat[:], [[-1, 128]], 1, ALU.is_ge, 0.0)
    memset("pool", mS[:], 1.0)
    asel(mS[:], [[-1, 128]], 1, ALU.is_gt, 0.0)
    memset("pool", mSp[:], 1.0)
    asel(mSp[:], [[1, 128]], -1, ALU.is_gt, 0.0)
    memset("pool", sel[:], 0.0)
    asel(sel[:], [[-1, NE], [0, 128]], 1, ALU.not_equal, 1.0)
    NEGB = -30000.0
    negU = CP.sb("negU", [128, 128], BF16)
    negV = CP.sb("negV", [128, 128], BF16)
    negtmp = CP.sb("negtmp", [128, 128])
    memset("pool", negtmp[:], NEGB)
    asel(negtmp[:], [[-1, 128]], 1, ALU.is_gt, 0.0)
    S.op("dve", "tensor_copy", out=negU[:], in_=negtmp[:])
    memset("pool", negtmp[:], NEGB)
    asel(negtmp[:], [[1, 128]], -1, ALU.is_gt, 0.0)
    S.op("dve", "tensor_copy", out=negV[:], in_=negtmp[:])
    S.dma("sp", msk[:], msk_in[0:1, :].partition_broadcast(128))
    S.op("dve", "tensor_scalar", out=omsk[:], in0=msk[:], scalar1=-1.0, scalar2=1.0, op0=ALU.mult, op1=ALU.add)

    def rstd_from_ssq(P, ssq, n, inv_n, eps):
        S.op("act", "activation", out=ssq[:, 0:n], in_=ssq[:, 0:n], func=AF.Ln, bias=eps, scale=inv_n)
        S.op("act", "activation", out=ssq[:, 0:n], in_=ssq[:, 0:n], func=AF.Exp, scale=-0.5)

    def load_mod_bcast(P, l, s_idx, which, name):
        b = P.sb(name, [128, D])
        S.dma("sp", b[:], modd[l, s_idx:s_idx + 1, which * D:(which + 1) * D].partition_broadcast(128))
        return b

    def load_vec_bcast(P, ap2d, n, name):
        b = P.sb(name, [128, n])
        S.dma("sp", b[:], ap2d.partition_broadcast(128))
        return b

    def phase_copy_in():
        P = Pool()
        for src, dst, T in ((x_in, streams["lat"]["h"], L), (ctx_in, streams["ctx"]["h"], CTX)):
            nt = T // 128
            step = min(nt, 8)
            bufs = [P.sb("cp", [128, step, D]) for _ in range(2)]
            for i, t0 in enumerate(range(0, nt, step)):
                b = bufs[i % 2]
                sv = src[t0 * 128:(t0 + step) * 128, :].rearrange("(j p) d -> p j d", p=128)
                dv = dst[t0 * 128:(t0 + step) * 128, :].rearrange("(j p) d -> p j d", p=128)
                S.dma("sp", b[:], sv)
                S.dma("pool", dv, b[:])
        P.close()

    def phase_ada(l):
        P = Pool()
        cv = P.sb("cv", [128, 8, 2])
        S.dma("sp", cv[:], cvT.rearrange("(c p) s -> p c s", p=128))
        S.op("act", "activation", out=cv[:], in_=cv[:], func=AF.Silu)
        bb = P.sb("bb", [2, 6 * D])
        S.dma("sp", bb[:], b_ada[l:l + 1, :].partition_broadcast(2))
        mo = P.sb("mo", [2, 6 * D])
        wts = [P.sb("wada", [128, 8, 512]) for _ in range(2)]
        pss = [P.ps("psa", [128, 512]) for _ in range(2)]
        for cb in range(12):
            wt = wts[cb % 2]
            ps = pss[cb % 2]
            S.dma("sp", wt[:], w_ada[l, :, cb * 512:(cb + 1) * 512].rearrange("(c p) n -> p c n", p=128))
            for kc in range(8):
                S.op("pe", "matmul", out=ps[0:2, :], lhsT=cv[:, kc, :], rhs=wt[:, kc, :], start=(kc == 0), stop=(kc == 7))
            S.op("dve", "tensor_tensor", out=mo[:, cb * 512:(cb + 1) * 512], in0=ps[0:2, :], in1=bb[:, cb * 512:(cb + 1) * 512], op=ALU.add)
        S.dma("pool", modd[l], mo[:])
        P.close()

    def phase_wprep(l):
        P = Pool()
        st = [P.sb("wst", [128, 8, FF]) for _ in range(2)]
        ob = [P.sb("wob", [128, 8, 2 * FF], BF16) for _ in range(2)]
        st2 = [P.sb("wst2", [128, 2, D]) for _ in range(2)]
        ob2 = [P.sb("wob2", [128, 2, D], BF16) for _ in range(2)]
        k = 0
        for e in range(NE):
            o = ob[e % 2]
            for j, w in enumerate((w_gate, w_up)):
                s_ = st[k % 2]
                k += 1
                S.dma("sp", s_[:], w[l, e].rearrange("(c p) f -> p c f", p=128))
                cast("act" if j == 0 else "dve", o[:, :, j * FF:(j + 1) * FF], s_[:])
            S.dma("pool", wgu_bf[e], o[:])
            s2 = st2[e % 2]
            o2 = ob2[e % 2]
            S.dma("sp", s2[:], w_down[l, e].rearrange("(c p) d -> p c d", p=128))
            S.op("dve", "tensor_copy", out=o2[:], in_=s2[:])
            S.dma("pool", wd_bf[e], o2[:])
        P.close()

    def phase_p1(l, sname):
        st = streams[sname]
        T = st["T"]
        s_idx = 0 if sname == "lat" else 1
        TT = min(512, T)
        nsub = TT // 128
        P = Pool()
        G1 = load_mod_bcast(P, l, s_idx, 1, "G1")
        S1 = load_mod_bcast(P, l, s_idx, 0, "S1")
        gm = load_vec_bcast(P, g_mix[l:l + 1, :], D, "gm")
        S.op("dve", "scalar_tensor_tensor", out=G1[:], in0=G1[:], scalar=1.0, in1=gm[:], op0=ALU.add, op1=ALU.mult)
        dtb = load_vec_bcast(P, dt_bias[l:l + 1, :], 16, "dtb")
        wbf = P.sb("wbf", [128, 8, DIN], BF16)
        wst = [P.sb("wst", [128, 8, 368]) for _ in range(2)]
        for i in range(7):
            c0 = i * 368
            s_ = wst[i % 2]
            S.dma("sp", s_[:], w_in[l, :, c0:c0 + 368].rearrange("(c p) n -> p c n", p=128))
            cast("act" if i % 2 else "dve", wbf[:, :, c0:c0 + 368], s_[:])
        ht = [P.sb("ht", [128, nsub, D]) for _ in range(2)]
        junk = P.sb("junk", [128, D])
        ssq = P.sb("ssq", [128, 4])
        hx = P.sb("hx", [128, D], BF16)
        tmp = P.sb("tmp", [128, D])
        hxTs = [P.sb("hxT", [128, 8, TT], BF16) for _ in range(2)]
        pT = P.ps("pT", [128, 8 * 128], BF16)
        psA = [P.ps("psA", [128, 512]) for _ in range(2)]
        psB = [P.ps("psB", [128, 512]) for _ in range(2)]
        psD = P.ps("psD", [128, 16])
        sg = P.sb("sg", [128, 512])
        v_sb = [P.sb("v_sb", [128, 512], BF16) for _ in range(2)]
        xb_sb = [P.sb("xb_sb", [128, 512], BF16) for _ in range(2)]
        sz_sb = P.sb("sz_sb", [128, nsub, 512])
        dt_sb = P.sb("dt_sb", [128, nsub, 16])
        def stage_a(ti):
            t0 = ti * TT
            h_ = ht[ti % 2]
            hxT = hxTs[ti % 2]
            S.dma("sp", h_[:], st["h"][t0:t0 + TT, :].rearrange("(j p) d -> p j d", p=128))
            for j in range(nsub):
                S.op("act", "activation", out=junk[:], in_=h_[:, j, :], func=AF.Square, accum_out=ssq[:, j:j + 1])
            rstd_from_ssq(P, ssq, nsub, 1.0 / D, RMS_EPS)
            for j in range(nsub):
                S.op("dve", "scalar_tensor_tensor", out=tmp[:], in0=h_[:, j, :], scalar=ssq[:, j:j + 1], in1=G1[:], op0=ALU.mult, op1=ALU.mult)
                S.op("dve", "tensor_tensor", out=hx[:], in0=tmp[:], in1=S1[:], op=ALU.add)
                for kc in range(8):
                    S.op("pe", "transpose", out=pT[:, kc * 128:(kc + 1) * 128], in_=hx[:, kc * 128:(kc + 1) * 128], identity=ident_b[:])
                S.op("act", "activation", out=hxT[:, :, j * 128:(j + 1) * 128], in_=pT[:].rearrange("p (c t) -> p c t", c=8), func=AF.Copy)

        def stage_b(ti):
            t0 = ti * TT
            hxT = hxTs[ti % 2]
            for c in range(4):
                pu, pg = psA[c % 2], psB[c % 2]
                for kc in range(8):
                    S.op("pe", "matmul", _ni=True, out=pu[:, 0:TT], lhsT=wbf[:, kc, c * 128:(c + 1) * 128], rhs=hxT[:, kc, :], start=(kc == 0), stop=(kc == 7))
                for kc in range(8):
                    S.op("pe", "matmul", _ni=True, out=pg[:, 0:TT], lhsT=wbf[:, kc, 512 + c * 128:512 + (c + 1) * 128], rhs=hxT[:, kc, :], start=(kc == 0), stop=(kc == 7))
                S.op("act", "activation", out=sg[:, 0:TT], in_=pg[:, 0:TT], func=AF.Sigmoid)
                vb = v_sb[c % 2]
                S.op("dve", "tensor_tensor", out=vb[:, 0:TT], in0=pu[:, 0:TT], in1=sg[:, 0:TT], op=ALU.mult)
                S.dma("pool", st["vT"][c * 128:(c + 1) * 128, t0:t0 + TT], vb[:, 0:TT])
            for c in range(8):
                pu = (psA + psB)[c % 4]
                col = 1536 + c * 128
                for kc in range(8):
                    S.op("pe", "matmul", _ni=True, out=pu[:, 0:TT], lhsT=wbf[:, kc, col:col + 128], rhs=hxT[:, kc, :], start=(kc == 0), stop=(kc == 7))
                xb = xb_sb[c % 2]
                S.op("act", "activation", out=xb[:, 0:TT], in_=pu[:, 0:TT], func=AF.Copy)
                S.dma("pool", st["xbcT"][c * 128:(c + 1) * 128, t0:t0 + TT], xb[:, 0:TT])
            for j in range(nsub):
                pz = psA[j % 2]
                for kc in range(8):
                    S.op("pe", "matmul", _ni=True, out=pz[:], lhsT=hxT[:, kc, j * 128:(j + 1) * 128], rhs=wbf[:, kc, 1024:1536], start=(kc == 0), stop=(kc == 7))
                for kc in range(8):
                    S.op("pe", "matmul", _ni=True, out=psD[:], lhsT=hxT[:, kc, j * 128:(j + 1) * 128], rhs=wbf[:, kc, 2560:2576], start=(kc == 0), stop=(kc == 7))
                S.op("act", "activation", out=sz_sb[:, j, :], in_=pz[:], func=AF.Silu)
                S.op("dve", "tensor_tensor", out=dt_sb[:, j, :], in0=psD[:], in1=dtb[:], op=ALU.add)
            S.op("act", "activation", out=dt_sb[:], in_=dt_sb[:], func=AF.Exp)
            S.op("act", "activation", out=dt_sb[:], in_=dt_sb[:], func=AF.Ln, bias=1.0, scale=1.0)
            S.dma("pool", st["sz"][t0:t0 + TT, :].rearrange("(j p) d -> p j d", p=128), sz_sb[:])
            S.dma("pool", st["dt"][t0:t0 + TT, :].rearrange("(j p) d -> p j d", p=128), dt_sb[:])

        ntile = T // TT
        stage_a(0)
        for ti in range(ntile):
            if ti + 1 < ntile:
                stage_a(ti + 1)
            stage_b(ti)
        P.close()

    def phase_p2(l, sname):
        st = streams[sname]
        T = st["T"]
        grid = sname == "lat"
        P = Pool()
        cw = P.sb("cw", [128, 4, 31])
        S.dma("sp", cw[:], conv_wT[l])
        cpc = P.sb("cpc", [128, 3, 4])
        S.dma("sp", cpc[:], conv_pc[l])
        dg = P.sb("dg", [128, 4, 31, 128], BF16)
        for c in range(4):
            S.op("dve", "tensor_tensor", out=dg[:, c, :, :], in0=ident_f[:].unsqueeze(1).to_broadcast([128, 31, 128]),
                 in1=cw[:, c, :].unsqueeze(2).to_broadcast([128, 31, 128]), op=ALU.mult)
        NT = 512 if grid else T
        acc = [P.sb("acc", [128, NT]) for _ in range(4)]
        sq = [P.sb("sq", [128, NT]) for _ in range(2)]
        pcs = [P.ps("pc", [128, 512]) for _ in range(3)]
        ps1 = P.ps("ps1", [128, 512])
        ps2 = P.ps("ps2", [128, 512])
        mean = P.sb("mean", [128, NT])
        var = P.sb("var", [128, NT])
        tnorm = [P.sb("tnorm", [128, NT]) for _ in range(2)]
        co = [P.sb("co", [128, NT], BF16) for _ in range(2)]
        if grid:
            RT = 8
            wcol = [P.sb("wcol", [128, RT, 94], BF16) for _ in range(3)]
            for w_ in wcol:
                memset("dve", w_[:], 0.0)
            wrow = [P.sb("wrow", [128, RT + 30, 64], BF16) for _ in range(3)]
        else:
            wseq = [P.sb("wseq", [128, T + 30], BF16) for _ in range(2)]
        ntiles = T // NT
        k = 0
        kr = 0
        ip = 0
        for ti in range(ntiles):
            t0 = ti * NT
            for c in range(4):
                a_ = acc[c]
                pc_ = pcs[ip % 3]
                ip += 1
                if grid and c < 2:
                    w_ = wcol[k % 3]
                    k += 1
                    S.dma("sp", w_[:, :, 15:79], st["vT"][c * 128:(c + 1) * 128, t0:t0 + NT].rearrange("p (r w) -> p r w", w=64))
                    for j in range(31):
                        S.op("pe", "matmul", out=pc_[:].rearrange("p (r w) -> p r w", w=64), lhsT=dg[:, c, j, :], rhs=w_[:, :, j:j + 64],
                             start=(j == 0), stop=(j == 30))
                else:
                    if grid:
                        r0 = t0 // 64
                        w_ = wrow[kr % 3]
                        kr += 1
                        lo_r, hi_r = r0 - 15, r0 + RT + 15
                        vlo, vhi = max(lo_r, 0), min(hi_r, ROWS)
                        if (vlo > lo_r or vhi < hi_r) and not multi:
                            memset("dve", w_[:], 0.0)
                        S.dma("sp", w_[:, vlo - lo_r:vhi - lo_r, :], st["vT"][c * 128:(c + 1) * 128, vlo * 64:vhi * 64].rearrange("p (r w) -> p r w", w=64))
                        if multi and lo_r < 0:
                            n_ = -lo_r
                            src = haloL[:, 1920 + (c - 2) * 960:1920 + (c - 1) * 960].rearrange("p (r w) -> p r w", w=64)
                            S.dma("sp", w_[:, 0:n_, :], src[:, 15 - n_:15, :])
                        if multi and hi_r > ROWS:
                            n_ = hi_r - ROWS
                            src = haloR[:, (c - 2) * 960:(c - 1) * 960].rearrange("p (r w) -> p r w", w=64)
                            S.dma("sp", w_[:, ROWS - lo_r:ROWS - lo_r + n_, :], src[:, 0:n_, :])
                        wf = w_[:].rearrange("p r w -> p (r w)")
                        step = 64
                    else:
                        w_ = wseq[k % 2]
                        k += 1
                        memset("dve", w_[:], 0.0)
                        S.dma("sp", w_[:, 15:15 + T], st["vT"][c * 128:(c + 1) * 128, :])
                        wf = w_[:]
                        step = 1
                    for j in range(31):
                        S.op("pe", "matmul", out=pc_[:, 0:NT], lhsT=dg[:, c, j, :], rhs=wf[:, j * step:j * step + NT], start=(j == 0), stop=(j == 30))
                S.op("dve", "tensor_scalar", out=a_[:], in0=pc_[:, 0:NT], scalar1=cpc[:, 0, c:c + 1], scalar2=None, op0=ALU.add)
            for c in range(4):
                S.op("pe", "matmul", out=ps1[:, 0:NT], lhsT=ones_f[:], rhs=acc[c][:], start=(c == 0), stop=(c == 3))
            for c in range(4):
                q_ = sq[c % 2]
                S.op("act", "activation", out=q_[:], in_=acc[c][:], func=AF.Square)
                S.op("pe", "matmul", out=ps2[:, 0:NT], lhsT=ones_f[:], rhs=q_[:], start=(c == 0), stop=(c == 3))
            S.op("act", "activation", out=mean[:], in_=ps1[:, 0:NT], func=AF.Copy, scale=1.0 / 512)
            S.op("dve", "tensor_tensor", out=var[:], in0=mean[:], in1=mean[:], op=ALU.mult)
            S.op("dve", "scalar_tensor_tensor", out=var[:], in0=ps2[:, 0:NT], scalar=1.0 / 512, in1=var[:], op0=ALU.mult, op1=ALU.subtract)
            S.op("act", "activation", out=var[:], in_=var[:], func=AF.Ln, bias=LN_EPS, scale=1.0)
            S.op("act", "activation", out=var[:], in_=var[:], func=AF.Exp, scale=-0.5)
            for c in range(4):
                tn = tnorm[c % 2]
                S.op("dve", "tensor_tensor", out=tn[:], in0=acc[c][:], in1=mean[:], op=ALU.subtract)
                S.op("dve", "tensor_tensor", out=tn[:], in0=tn[:], in1=var[:], op=ALU.mult)
                o = co[c % 2]
                S.op("act", "activation", out=o[:], in_=tn[:], func=AF.Silu, bias=cpc[:, 2, c:c + 1], scale=cpc[:, 1, c:c + 1])
                S.dma("pool", st["coT"][c * 128:(c + 1) * 128, t0:t0 + NT], o[:])
        P.close()

    def phase_p2b(l, sname):
        st = streams[sname]
        T = st["T"]
        TT = min(512, T)
        nsub = TT // 128
        P = Pool()
        sw = P.sb("sw", [128, 8, 5])
        S.dma("sp", sw[:], sconv_wT[l])
        sb_ = P.sb("sb_", [128, 8])
        S.dma("sp", sb_[:], sconv_b[l])
        dg = P.sb("dg5", [128, 8, 5, 128], BF16)
        for c in range(8):
            S.op("dve", "tensor_tensor", out=dg[:, c, :, :], in0=ident_f[:].unsqueeze(1).to_broadcast([128, 5, 128]),
                 in1=sw[:, c, :].unsqueeze(2).to_broadcast([128, 5, 128]), op=ALU.mult)
        win = [P.sb("win", [128, 8, TT + 4], BF16) for _ in range(2)]
        xc = [P.sb("xc", [128, 8, TT], BF16) for _ in range(2)]
        pcs = [P.ps("pc", [128, 512]) for _ in range(3)]
        pT = P.ps("pT", [128, 768], BF16)
        tok = [P.sb("tok", [128, 768], BF16) for _ in range(2)]
        ntiles = T // TT
        q = 0
        ip = 0
        for ti in range(ntiles):
            t0 = ti * TT
            w_ = win[ti % 2]
            lo, hi = t0 - 2, t0 + TT + 2
            vlo, vhi = max(lo, 0), min(hi, T)
            use_halo = multi and sname == "lat"
            if (vlo > lo or vhi < hi) and not use_halo:
                memset("dve", w_[:], 0.0)
            S.dma("sp", w_[:, :, vlo - lo:vhi - lo], st["xbcT"][:, vlo:vhi].rearrange("(c p) t -> p c t", p=128))
            if use_halo and lo < 0:
                S.dma("sp", w_[:, :, 0:2], haloL[:, 3840:3872].rearrange("p (c t) -> p c t", t=4)[:, :, 2:4])
            if use_halo and hi > T:
                S.dma("sp", w_[:, :, TT + 2:TT + 4], haloR[:, 3840:3872].rearrange("p (c t) -> p c t", t=4)[:, :, 0:2])
            x_ = xc[ti % 2]
            for c in range(8):
                pc_ = pcs[ip % 3]
                ip += 1
                for j in range(5):
                    S.op("pe", "matmul", out=pc_[:, 0:TT], lhsT=dg[:, c, j, :], rhs=w_[:, c, j:j + TT], start=(j == 0), stop=(j == 4))
                S.op("act", "activation", out=x_[:, c, :], in_=pc_[:, 0:TT], func=AF.Silu, bias=sb_[:, c:c + 1], scale=1.0)
            S.dma("pool", st["xcT"][:, t0:t0 + TT].rearrange("(c p) t -> p c t", p=128), x_[:, 4:8, :])
            for j in range(nsub):
                for c in range(6):
                    S.op("pe", "transpose", out=pT[:, c * 128:(c + 1) * 128], in_=x_[:, c, j * 128:(j + 1) * 128], identity=ident_b[:])
                tk = tok[q % 2]
                q += 1
                S.op("act", "activation", out=tk[:], in_=pT[:], func=AF.Copy)
                S.dma("pool", st["xbtok"][t0 + j * 128:t0 + (j + 1) * 128, :], tk[:])
        P.close()

    def ssd_consts(P, l):
        al = load_vec_bcast(P, a_log[l:l + 1, :], 16, "al")
        S.op("act", "activation", out=al[:], in_=al[:], func=AF.Exp)
        S.op("dve", "tensor_scalar", out=al[:], in0=al[:], scalar1=-1.0, scalar2=None, op0=ALU.mult)
        return al

    def chunk_decays(P, W, dt_c, a_bc, cum_ps):
        S.op("dve", "tensor_tensor", out=W["dta"][:], in0=dt_c[:], in1=a_bc[:], op=ALU.mult)
        S.op("pe", "matmul", out=cum_ps[:, 0:8], lhsT=Umat[:], rhs=W["dta"][:, 0:8], start=True, stop=True)
        S.op("pe", "matmul", out=cum_ps[:, 8:16], lhsT=Vmat[:], rhs=W["dta"][:, 8:16], start=True, stop=True)
        S.op("pe", "matmul", out=cum_ps[:, 16:32], lhsT=ones_f[:], rhs=W["dta"][:], start=True, stop=True)
        S.op("act", "activation", out=W["cum"][:], in_=cum_ps[:, 0:32], func=AF.Copy)
        S.op("act", "activation", out=W["E1"][:], in_=W["cum"][:, 0:16], func=AF.Exp)
        S.op("act", "activation", out=W["dtot"][:], in_=W["cum"][:, 16:32], func=AF.Exp)
        S.op("dve", "tensor_tensor", out=W["wgt"][:], in0=W["cum"][:, 16:32], in1=W["cum"][:, 0:16], op=ALU.subtract)
        S.op("act", "activation", out=W["wgt"][:], in_=W["wgt"][:], func=AF.Exp)
        S.op("dve", "tensor_tensor", out=W["wgt"][:], in0=W["wgt"][:], in1=dt_c[:], op=ALU.mult)

    def decay_bufs(P):
        return dict(dta=P.sb("dta", [128, 16]), cum=P.sb("cum", [128, 32]), E1=P.sb("E1", [128, 16]),
                    dtot=P.sb("dtot", [128, 16]), wgt=P.sb("wgt", [128, 16]))

    def state_update(Hs, tok_, W, d, xw, S_ps, Hbf, e2="dve"):
        S.op(e2, "tensor_tensor", out=xw[:].rearrange("p (h q) -> p h q", h=8), in0=tok_[:, 0:512].rearrange("p (h q) -> p h q", h=8),
             in1=W["wgt"][:, d * 8:(d + 1) * 8].unsqueeze(2).to_broadcast([128, 8, 64]), op=ALU.mult)
        for g in range(2):
            S.op("pe", "matmul", out=S_ps[:, g * 256:(g + 1) * 256], lhsT=tok_[:, 512 + g * 128:512 + (g + 1) * 128],
                 rhs=xw[:, g * 256:(g + 1) * 256], start=True, stop=True)
        S.op(e2, "tensor_tensor", out=Hs[:].rearrange("p (h q) -> p h q", h=8), in0=Hs[:].rearrange("p (h q) -> p h q", h=8),
             in1=W["dtot"][:, d * 8:(d + 1) * 8].unsqueeze(2).to_broadcast([128, 8, 64]), op=ALU.mult)
        S.op("dve", "tensor_tensor", out=Hs[:], in0=Hs[:], in1=S_ps[:], op=ALU.add)
        if Hbf is not None:
            S.op("act", "activation", out=Hbf[:], in_=Hs[:], func=AF.Copy)

    def phase_state_sweep(l, sname, d, store, acc_decay=False):
        st = streams[sname]
        T = st["T"]
        nch = T // 128
        P = Pool()
        a_bc = ssd_consts(P, l)
        Ws = [decay_bufs(P), decay_bufs(P)]
        cum_ps = P.ps("cum_ps", [128, 32])
        S_ps = P.ps("S_ps", [128, 512])
        toks = [P.sb("tok", [128, 768], BF16) for _ in range(3)]
        dts = [P.sb("dtc", [128, 16]) for _ in range(3)]
        xws = [P.sb("xw", [128, 512], BF16) for _ in range(2)]
        Hbf = [P.sb("Hbf", [128, 512], BF16) for _ in range(2)]
        Hs = Hb if d == 1 else Hf
        order = list(range(nch - 1, -1, -1)) if d == 1 else list(range(nch))
        h8 = lambda r: r.rearrange("p (h q) -> p h q", h=8)

        def L(i):
            c = order[i]
            S.dma("sp", toks[i % 3][:], st["xbtok"][c * 128:(c + 1) * 128, :])
            S.dma("sp", dts[i % 3][:], st["dt"][c * 128:(c + 1) * 128, :])

        def D1(i):
            W, dtc = Ws[i % 2], dts[i % 3]
            S.op("dve", "tensor_tensor", out=W["dta"][:], in0=dtc[:], in1=a_bc[:], op=ALU.mult)
            S.op("pe", "matmul", out=cum_ps[:, 0:8], lhsT=Umat[:], rhs=W["dta"][:, 0:8], start=True, stop=True)
            S.op("pe", "matmul", out=cum_ps[:, 8:16], lhsT=Vmat[:], rhs=W["dta"][:, 8:16], start=True, stop=True)
            S.op("pe", "matmul", out=cum_ps[:, 16:32], lhsT=ones_f[:], rhs=W["dta"][:], start=True, stop=True)
            S.op("act", "activation", out=W["cum"][:], in_=cum_ps[:, 0:32], func=AF.Copy)
            S.op("act", "activation", out=W["dtot"][:], in_=W["cum"][:, 16:32], func=AF.Exp)

        def D2(i):
            W, dtc = Ws[i % 2], dts[i % 3]
            S.op("dve", "tensor_tensor", out=W["wgt"][:], in0=W["cum"][:, 16:32], in1=W["cum"][:, 0:16], op=ALU.subtract)
            S.op("act", "activation", out=W["wgt"][:], in_=W["wgt"][:], func=AF.Exp)
            S.op("dve", "tensor_tensor", out=W["wgt"][:], in0=W["wgt"][:], in1=dtc[:], op=ALU.mult)
            if acc_decay:
                S.op("dve", "tensor_tensor", out=Dacc[:], in0=Dacc[:], in1=W["dtot"][:], op=ALU.mult)

        def S1(i):
            c = order[i]
            W, tk, xw = Ws[i % 2], toks[i % 3], xws[i % 2]
            if store:
                hb = Hbf[i % 2]
                S.op("act", "activation", out=hb[:], in_=Hs[:], func=AF.Copy)
                S.dma("pool", st["Hbd"][c], hb[:])
            S.op("dve", "tensor_tensor", out=h8(xw[:]), in0=h8(tk[:, 0:512]),
                 in1=W["wgt"][:, d * 8:(d + 1) * 8].unsqueeze(2).to_broadcast([128, 8, 64]), op=ALU.mult)
            for g in range(2):
                S.op("pe", "matmul", out=S_ps[:, g * 256:(g + 1) * 256], lhsT=tk[:, 512 + g * 128:512 + (g + 1) * 128],
                     rhs=xw[:, g * 256:(g + 1) * 256], start=True, stop=True)

        def S2(i):
            W = Ws[i % 2]
            S.op("dve", "tensor_tensor", out=h8(Hs[:]), in0=h8(Hs[:]), in1=W["dtot"][:, d * 8:(d + 1) * 8].unsqueeze(2).to_broadcast([128, 8, 64]), op=ALU.mult)
            S.op("dve", "tensor_tensor", out=Hs[:], in0=Hs[:], in1=S_ps[:], op=ALU.add)

        L(0)
        if nch > 1:
            L(1)
        D1(0)
        D2(0)
        for i in range(nch):
            if i + 2 < nch:
                L(i + 2)
            if i + 1 < nch:
                D1(i + 1)
            S1(i)
            if i + 1 < nch:
                D2(i + 1)
            S2(i)
        P.close()

    def phase_exchange1():
        st = streams["lat"]
        P = Pool()
        pk = P.sb("pk", [128, W1], BF16)
        for c in (2, 3):
            S.dma("sp", pk[:, (c - 2) * 960:(c - 1) * 960], st["vT"][c * 128:(c + 1) * 128, 0:960])
            S.dma("sp", pk[:, 1920 + (c - 2) * 960:1920 + (c - 1) * 960], st["vT"][c * 128:(c + 1) * 128, L - 960:L])
        xv = pk[:, 3840:3872].rearrange("p (c t) -> p c t", t=4)
        S.dma("sp", xv[:, :, 0:2], st["xbcT"][:, 0:2].rearrange("(c p) t -> p c t", p=128))
        S.dma("sp", xv[:, :, 2:4], st["xbcT"][:, L - 2:L].rearrange("(c p) t -> p c t", p=128))
        S.dma("pool", send1, pk[:])
        S.collective(send1, recv1, ncores)
        accL = P.sb("accL", [128, W1], BF16)
        accR = P.sb("accR", [128, W1], BF16)
        rj = [P.sb("rj", [128, W1], BF16) for _ in range(2)]
        for j in range(ncores):
            r_ = rj[j % 2]
            S.dma("sp", r_[:], recv1[j * 128:(j + 1) * 128, :])
            if j == 0:
                S.op("dve", "tensor_scalar", out=accL[:], in0=r_[:], scalar1=msk[:, 0:1], scalar2=None, op0=ALU.mult)
                S.op("dve", "tensor_scalar", out=accR[:], in0=r_[:], scalar1=msk[:, 8:9], scalar2=None, op0=ALU.mult)
            else:
                S.op("dve", "scalar_tensor_tensor", out=accL[:], in0=r_[:], scalar=msk[:, j:j + 1], in1=accL[:], op0=ALU.mult, op1=ALU.add)
                S.op("dve", "scalar_tensor_tensor", out=accR[:], in0=r_[:], scalar=msk[:, 8 + j:9 + j], in1=accR[:], op0=ALU.mult, op1=ALU.add)
        S.dma("pool", haloL, accL[:])
        S.dma("pool", haloR, accR[:])
        P.close()

    def phase_exchange2():
        P = Pool()
        pk = P.sb("pk2", [128, W2])
        S.op("dve", "tensor_copy", out=pk[:, 0:512], in_=Hf[:])
        S.op("dve", "tensor_copy", out=pk[:, 512:1024], in_=Hb[:])
        S.op("dve", "tensor_copy", out=pk[:, 1024:1040], in_=Dacc[:])
        S.dma("pool", send2, pk[:])
        S.collective(send2, recv2, ncores)
        rj = [P.sb("rj2", [128, W2]) for _ in range(ncores)]
        for j in range(ncores):
            S.dma("sp", rj[j][:], recv2[j * 128:(j + 1) * 128, :])
        dsel = P.sb("dsel", [128, 8])
        S.op("dve", "tensor_copy", out=Hf[:], in_=hf0[:])
        S.op("dve", "tensor_copy", out=Hb[:], in_=hb0[:])
        h8 = lambda r: r.rearrange("p (h q) -> p h q", h=8)
        for d, Hs, order, mo in ((0, Hf, range(ncores), 16), (1, Hb, range(ncores - 1, -1, -1), 24)):
            for j in order:
                r_ = rj[j]
                S.op("dve", "tensor_scalar", out=dsel[:], in0=r_[:, 1024 + d * 8:1032 + d * 8], scalar1=msk[:, mo + j:mo + j + 1],
                     scalar2=omsk[:, mo + j:mo + j + 1], op0=ALU.mult, op1=ALU.add)
                S.op("dve", "tensor_tensor", out=h8(Hs[:]), in0=h8(Hs[:]), in1=dsel[:].unsqueeze(2).to_broadcast([128, 8, 64]), op=ALU.mult)
                S.op("dve", "scalar_tensor_tensor", out=Hs[:], in0=r_[:, d * 512:(d + 1) * 512], scalar=msk[:, mo + j:mo + j + 1], in1=Hs[:],
                     op0=ALU.mult, op1=ALU.add)
        P.close()

    def phase_p4(l, sname):
        st = streams[sname]
        T = st["T"]
        s_idx = 0 if sname == "lat" else 1
        nch = T // 128
        P = Pool()
        a_bc = ssd_consts(P, l)
        W = decay_bufs(P)
        dsk = load_vec_bcast(P, d_skip[l:l + 1, :], 8, "dsk")
        gs = load_vec_bcast(P, ssd_g[l:l + 1, :], 512, "gs")
        GT1 = load_mod_bcast(P, l, s_idx, 2, "GT1")
        wo = P.sb("wo", [128, 8, D], BF16)
        wst = [P.sb("wst", [128, 8, 256]) for _ in range(2)]
        for i in range(4):
            s_ = wst[i % 2]
            S.dma("sp", s_[:], w_out[l, :, i * 256:(i + 1) * 256].rearrange("(c p) n -> p c n", p=128))
            S.op("dve", "tensor_copy", out=wo[:, :, i * 256:(i + 1) * 256], in_=s_[:])
        toks = [P.sb("tok", [128, 768], BF16) for _ in range(3)]
        bcts = [P.sb("bct", [128, 4, 128], BF16) for _ in range(3)]
        dts = [P.sb("dtc", [128, 16]) for _ in range(3)]
        szs = [P.sb("szc", [128, 512]) for _ in range(3)]
        hbs = [P.sb("hbin", [128, 512], BF16) for _ in range(3)]
        cos = [P.sb("coc", [128, 4, 128], BF16) for _ in range(3)]
        hrs = [P.sb("hres", [128, D]) for _ in range(3)]
        Hfb = P.sb("Hfb", [128, 512], BF16)
        xw = P.sb("xw", [128, 512], BF16)
        A_f = P.sb("A_f", [128, 8, 128])
        A_b = P.sb("A_b", [128, 8, 128])
        E_f = P.sb("E_f", [128, 4, 128])
        E_b = P.sb("E_b", [128, 4, 128])
        MT = P.sb("MT", [128, 8, 128], BF16)
        t1 = P.sb("t1", [128, 512])
        t2 = P.sb("t2", [128, 512])
        t3 = P.sb("t3", [128, 512])
        lndt = P.sb("lndt", [128, 16])
        so = P.sb("so", [128, 512], BF16)
        ssq = P.sb("ssq", [128, 4])
        junk = P.sb("junk", [128, 512])
        ssdT = P.sb("ssdT", [128, 4, 128], BF16)
        P0 = P.ps("P0", [128, 512])
        P1 = P.ps("P1", [128, 512])
        P2 = P.ps("P2", [128, 512])
        P3 = P.ps("P3", [128, 512])
        P4 = P.ps("P4", [128, 512])
        P5 = P.ps("P5", [128, 512])
        P6 = P.ps("P6", [128, 512])
        P7 = P.ps("P7", [128, 512], BF16)
        h8 = lambda r: r.rearrange("p (h q) -> p h q", h=8)
        W2 = [W, decay_bufs(P), decay_bufs(P)]
        MTs = [MT, P.sb("MT2", [128, 8, 128], BF16), P.sb("MT3", [128, 8, 128], BF16)]

        def bufs(c):
            i = c % 3
            return toks[i], bcts[i], dts[i], szs[i], hbs[i], cos[i], hrs[i], W2[i], MTs[i]

        def load(c):
            tk, bct, dtc, szc, hbin, coc, hres, W, MT = bufs(c)
            tsl = slice(c * 128, (c + 1) * 128)
            S.dma("sp", tk[:], st["xbtok"][tsl, :])
            S.dma("sp", dtc[:], st["dt"][tsl, :])
            S.dma("sp", bct[:], st["xcT"][:, tsl].rearrange("(c p) t -> p c t", p=128))
            S.dma("sp", szc[:], st["sz"][tsl, :])
            S.dma("sp", hbin[:], st["Hbd"][c])
            S.dma("sp", coc[:], st["coT"][:, tsl].rearrange("(c p) t -> p c t", p=128))
            S.dma("sp", hres[:], st["h"][tsl, :])

        def seg_mm(c, g):
            tk, bct, dtc, szc, hbin, coc, hres, W, MT = bufs(c)
            for hh in range(4):
                h = g * 4 + hh
                S.op("pe", "matmul", out=P1[:, hh * 128:(hh + 1) * 128], lhsT=A_f[:, h, :], rhs=Umat[:], start=True, stop=False)
                S.op("pe", "matmul", out=P1[:, hh * 128:(hh + 1) * 128], lhsT=ident_b[:], rhs=negU[:], start=False, stop=True)
                S.op("pe", "matmul", out=P2[:, hh * 128:(hh + 1) * 128], lhsT=A_b[:, h, :], rhs=Vmat[:], start=True, stop=False)
                S.op("pe", "matmul", out=P2[:, hh * 128:(hh + 1) * 128], lhsT=ident_b[:], rhs=negV[:], start=False, stop=True)

        def seg_exp(c, g):
            for hh in range(4):
                h = g * 4 + hh
                S.op("act", "activation", out=E_f[:, hh, :], in_=P1[:, hh * 128:(hh + 1) * 128], func=AF.Exp, bias=lndt[:, h:h + 1], scale=1.0)
                S.op("act", "activation", out=E_b[:, hh, :], in_=P2[:, hh * 128:(hh + 1) * 128], func=AF.Exp, bias=lndt[:, 8 + h:9 + h], scale=1.0)

        def seg_mt(c, g):
            tk, bct, dtc, szc, hbin, coc, hres, W, MT = bufs(c)
            S.op("dve", "tensor_tensor", out=E_f[:], in0=E_f[:], in1=E_b[:], op=ALU.add)
            S.op("dve", "tensor_tensor", out=MT[:, g * 4:(g + 1) * 4, :], in0=E_f[:],
                 in1=P3[:, g * 128:(g + 1) * 128].unsqueeze(1).to_broadcast([128, 4, 128]), op=ALU.mult)

        def F0(c):
            tk, bct, dtc, szc, hbin, coc, hres, W, MT = bufs(c)
            chunk_decays(P, W, dtc, a_bc, P0)
            S.op("act", "activation", out=lndt[:], in_=dtc[:], func=AF.Ln)
            for g in range(2):
                S.op("pe", "matmul", out=P3[:, g * 128:(g + 1) * 128], lhsT=bct[:, g, :], rhs=bct[:, 2 + g, :], start=True, stop=True)

        def F1(c):
            tk, bct, dtc, szc, hbin, coc, hres, W, MT = bufs(c)
            S.op("dve", "tensor_tensor", out=A_f[:], in0=mS[:].unsqueeze(1).to_broadcast([128, 8, 128]),
                 in1=W["dta"][:, 0:8].unsqueeze(2).to_broadcast([128, 8, 128]), op=ALU.mult)
            S.op("dve", "tensor_tensor", out=A_b[:], in0=mSp[:].unsqueeze(1).to_broadcast([128, 8, 128]),
                 in1=W["dta"][:, 8:16].unsqueeze(2).to_broadcast([128, 8, 128]), op=ALU.mult)

        def F2(c):
            seg_mm(c, 0)

        def F3(c):
            seg_exp(c, 0)
            seg_mm(c, 1)

        def F4(c):
            seg_mt(c, 0)
            seg_exp(c, 1)

        def F5(c):
            seg_mt(c, 1)

        def B0(c):
            tk, bct, dtc, szc, hbin, coc, hres, W, MT = bufs(c)
            S.op("act", "activation", out=Hfb[:], in_=Hf[:], func=AF.Copy)
            for h in range(8):
                S.op("pe", "matmul", out=P4[:, h * 64:(h + 1) * 64], lhsT=MT[:, h, :], rhs=tk[:, h * 64:(h + 1) * 64], start=True, stop=True)
            for g in range(2):
                S.op("pe", "matmul", out=P5[:, g * 256:(g + 1) * 256], lhsT=bct[:, 2 + g, :], rhs=Hfb[:, g * 256:(g + 1) * 256], start=True, stop=True)
                S.op("pe", "matmul", out=P6[:, g * 256:(g + 1) * 256], lhsT=bct[:, 2 + g, :], rhs=hbin[:, g * 256:(g + 1) * 256], start=True, stop=True)
            S.op("dve", "tensor_tensor", out=h8(t3[:]), in0=h8(tk[:, 0:512]), in1=dsk[:].unsqueeze(2).to_broadcast([128, 8, 64]), op=ALU.mult)
            S.op("dve", "tensor_tensor", out=h8(t1[:]), in0=h8(P5[:]), in1=W["E1"][:, 0:8].unsqueeze(2).to_broadcast([128, 8, 64]), op=ALU.mult)
            S.op("dve", "tensor_tensor", out=h8(t2[:]), in0=h8(P6[:]), in1=W["E1"][:, 8:16].unsqueeze(2).to_broadcast([128, 8, 64]), op=ALU.mult)

        def B1(c):
            tk, bct, dtc, szc, hbin, coc, hres, W, MT = bufs(c)
            tsl = slice(c * 128, (c + 1) * 128)
            S.op("dve", "tensor_tensor", out=t1[:], in0=t1[:], in1=t2[:], op=ALU.add)
            S.op("dve", "tensor_tensor", out=t1[:], in0=t1[:], in1=P4[:], op=ALU.add)
            S.op("dve", "tensor_tensor", out=t1[:], in0=t1[:], in1=t3[:], op=ALU.add)
            if "dbg_y" in dbg:
                S.dma("pool", dbg_y[sname][tsl, :], t1[:])

        def B2(c):
            tk, bct, dtc, szc, hbin, coc, hres, W, MT = bufs(c)
            state_update(Hf, tk, W, 0, xw, P5, None, e2="dve")
            S.op("dve", "tensor_tensor", out=t1[:], in0=t1[:], in1=szc[:], op=ALU.mult)

        def B3(c):
            S.op("act", "activation", out=junk[:], in_=t1[:], func=AF.Square, accum_out=ssq[:, 0:1])
            rstd_from_ssq(P, ssq, 1, 1.0 / 512, RMS_EPS)
            S.op("dve", "scalar_tensor_tensor", out=so[:], in0=t1[:], scalar=ssq[:, 0:1], in1=gs[:], op0=ALU.mult, op1=ALU.mult)
            for k4 in range(4):
                S.op("pe", "transpose", out=P7[:, k4 * 128:(k4 + 1) * 128], in_=so[:, k4 * 128:(k4 + 1) * 128], identity=ident_b[:])
            S.op("act", "activation", out=ssdT[:].rearrange("p c t -> p (c t)"), in_=P7[:], func=AF.Copy)

        def outproj(c, half, PO, tq):
            tk, bct, dtc, szc, hbin, coc, hres, W, MT = bufs(c)
            for kc in range(8):
                lh = coc[:, kc, :] if kc < 4 else ssdT[:, kc - 4, :]
                S.op("pe", "matmul", _ni=True, out=PO[:], lhsT=lh, rhs=wo[:, kc, half * 512:(half + 1) * 512], start=(kc == 0), stop=(kc == 7))
            S.op("dve", "tensor_tensor", out=tq[:], in0=PO[:], in1=GT1[:, half * 512:(half + 1) * 512], op=ALU.mult)
            S.op("dve", "tensor_tensor", out=hres[:, half * 512:(half + 1) * 512], in0=hres[:, half * 512:(half + 1) * 512], in1=tq[:], op=ALU.add)

        def B4(c):
            outproj(c, 0, P6, t2)

        def B5(c):
            tk, bct, dtc, szc, hbin, coc, hres, W, MT = bufs(c)
            tsl = slice(c * 128, (c + 1) * 128)
            outproj(c, 1, P4, t3)
            S.dma("pool", st["h"][tsl, :], hres[:])

        FS = [F0, F1, F2, F3, F4, F5]
        BS = [B0, B1, B2, B3, B4, B5]
        load(0)
        if nch > 1:
            load(1)
        for f in FS:
            f(0)
        for c in range(nch):
            if c + 2 < nch:
                load(c + 2)
            for k in range(6):
                if c + 1 < nch:
                    FS[k](c + 1)
                BS[k](c)
        P.close()

    def phase_p6(l, sname, final):
        st = streams[sname]
        T = st["T"]
        s_idx = 0 if sname == "lat" else 1
        TB = min(1024, T)
        TW = min(512, TB)
        nsubB = TB // 128
        nsubW = TW // 128
        nblk = T // TB
        P = Pool()
        G2 = load_mod_bcast(P, l, s_idx, 4, "G2")
        S2 = load_mod_bcast(P, l, s_idx, 3, "S2")
        GT2 = load_mod_bcast(P, l, s_idx, 5, "GT2")
        gf = load_vec_bcast(P, g_ffn[l:l + 1, :], D, "gf")
        S.op("dve", "scalar_tensor_tensor", out=G2[:], in0=G2[:], scalar=1.0, in1=gf[:], op0=ALU.add, op1=ALU.mult)
        if final:
            S.dma("sp", gf[:], g_final[0:1, :].partition_broadcast(128))
        brb = load_vec_bcast(P, b_r[l:l + 1, :], 20, "brb")
        wr = P.sb("wr", [128, 8, 20])
        S.dma("sp", wr[:], w_r[l].rearrange("(c p) n -> p c n", p=128))
        hts = [P.sb("ht", [128, D]) for _ in range(2)]
        hcs = [P.sb("hc", [128, D]) for _ in range(2)]
        junkA = P.sb("junkA", [128, D])
        junkC = P.sb("junkC", [128, D])
        ssqA = P.sb("ssqA", [128, 4])
        ssqC = P.sb("ssqC", [128, 4])
        h2 = P.sb("h2", [128, D])
        h2Tf = P.sb("h2Tf", [128, 8, 128])
        h2Ts = [P.sb("h2T", [128, 8, TB], BF16) for _ in range(2)]
        maccs = [P.sb("macc", [128, nsubB, D]) for _ in range(2)]
        combss = [P.sb("combs", [128, nsubB, 16]) for _ in range(2)]
        lgg = P.sb("lgg", [128, nsubB, 4])
        lge = P.sb("lge", [128, nsubB, 16])
        r_gmax = P.sb("r_gmax", [128, nsubB])
        r_gmask = P.sb("r_gmask", [128, nsubB, 4])
        r_gexp = P.sb("r_gexp", [128, nsubB, 4])
        r_gsum = P.sb("r_gsum", [128, nsubB])
        r_elm = P.sb("r_elm", [128, nsubB, 16])
        r_m1 = P.sb("r_m1", [128, nsubB])
        r_m2 = P.sb("r_m2", [128, nsubB])
        r_mask1 = P.sb("r_mask1", [128, nsubB, 16])
        r_mask2 = P.sb("r_mask2", [128, nsubB, 16])
        wgus = [P.sb("wgu", [128, 8, 2 * FF], BF16) for _ in range(2)]
        wds = [P.sb("wd", [128, 2, D], BF16) for _ in range(2)]
        sgs = [P.sb("sg", [128, 512]) for _ in range(2)]
        hTs = [P.sb("hT", [128, 2, 512], BF16) for _ in range(2)]
        Q = [P.ps("Q%d" % i, [128, 512]) for i in range(8)]
        pgs, pus, pos = [Q[0], Q[1]], [Q[3], Q[4]], [Q[5], Q[6]]
        qa, qb = Q[2], Q[7]

        def A0(bi, j):
            tsl = slice(bi * TB + j * 128, bi * TB + (j + 1) * 128)
            S.dma("sp", hts[j % 2][:], st["h"][tsl, :])

        def A1(bi, j):
            h_ = hts[j % 2]
            S.op("act", "activation", out=junkA[:], in_=h_[:], func=AF.Square, accum_out=ssqA[:, 0:1])
            rstd_from_ssq(P, ssqA, 1, 1.0 / D, RMS_EPS)
            S.op("dve", "scalar_tensor_tensor", out=h2[:], in0=h_[:], scalar=ssqA[:, 0:1], in1=G2[:], op0=ALU.mult, op1=ALU.mult)
            S.op("dve", "tensor_tensor", out=h2[:], in0=h2[:], in1=S2[:], op=ALU.add)

        def A2(bi, j):
            for hf_, q_ in ((0, qa), (1, qb)):
                for kc in range(4):
                    kk = hf_ * 4 + kc
                    S.op("pe", "transpose", out=q_[:, kc * 128:(kc + 1) * 128], in_=h2[:, kk * 128:(kk + 1) * 128], identity=ident_f[:])
            for hf_, q_ in ((0, qa), (1, qb)):
                S.op("act", "activation", out=h2Tf[:, hf_ * 4:(hf_ + 1) * 4, :].rearrange("p c t -> p (c t)"), in_=q_[:], func=AF.Copy)

        def A3(bi, j):
            h2T = h2Ts[bi % 2]
            S.op("dve", "tensor_copy", out=h2T[:, :, j * 128:(j + 1) * 128], in_=h2Tf[:])
            for kc in range(8):
                S.op("pe", "matmul", _ni=True, out=qa[:, 0:20], lhsT=h2Tf[:, kc, :], rhs=wr[:, kc, :], start=(kc == 0), stop=(kc == 7))
            S.op("dve", "tensor_tensor", out=lgg[:, j, :], in0=qa[:, 0:4], in1=brb[:, 0:4], op=ALU.add)
            S.op("dve", "tensor_tensor", out=lge[:, j, :], in0=qa[:, 4:20], in1=brb[:, 4:20], op=ALU.add)

        def A_stages(bi):
            out = [[lambda: A0(bi, 0)]]
            for j in range(nsubB):
                nxt = [lambda j=j: A0(bi, j + 1)] if j + 1 < nsubB else []
                out.append([lambda j=j: A1(bi, j)] + nxt)
                out.append([lambda j=j: A2(bi, j)])
                out.append([lambda j=j: A3(bi, j)])
            out.append([lambda: router(bi)])
            return out

        def router(bi):
            combs = combss[bi % 2]
            V_ = lambda name, **kw: S.op("dve", name, **kw)
            bc = lambda r, n: r.unsqueeze(2).to_broadcast([128, r.ap.shape[1], n])
            V_("tensor_reduce", out=r_gmax[:], in_=lgg[:], axis=AX.X, op=ALU.max)
            V_("tensor_tensor", out=r_gmask[:], in0=lgg[:], in1=bc(r_gmax[:], 4), op=ALU.is_ge)
            V_("tensor_tensor", out=r_gexp[:], in0=lgg[:], in1=bc(r_gmax[:], 4), op=ALU.subtract)
            S.op("act", "activation", out=r_gexp[:], in_=r_gexp[:], func=AF.Exp)
            V_("tensor_reduce", out=r_gsum[:], in_=r_gexp[:], axis=AX.X, op=ALU.add)
            V_("reciprocal", out=r_gsum[:], in_=r_gsum[:])
            V_("tensor_scalar", out=r_gmask[:], in0=r_gmask[:], scalar1=-1.0, scalar2=-NEG, op0=ALU.add, op1=ALU.mult)
            pen = r_gmask[:].rearrange("p j g -> p (j g)")
            V_("tensor_tensor", out=r_elm[:].rearrange("p j (g e) -> p (j g) e", g=4), in0=lge[:].rearrange("p j (g e) -> p (j g) e", g=4),
               in1=bc(pen, 4), op=ALU.add)
            V_("tensor_reduce", out=r_m1[:], in_=r_elm[:], axis=AX.X, op=ALU.max)
            V_("tensor_tensor", out=r_mask1[:], in0=r_elm[:], in1=bc(r_m1[:], 16), op=ALU.is_ge)
            V_("scalar_tensor_tensor", out=r_elm[:], in0=r_mask1[:], scalar=NEG, in1=r_elm[:], op0=ALU.mult, op1=ALU.add)
            V_("tensor_reduce", out=r_m2[:], in_=r_elm[:], axis=AX.X, op=ALU.max)
            V_("tensor_tensor", out=r_mask2[:], in0=r_elm[:], in1=bc(r_m2[:], 16), op=ALU.is_ge)
            V_("tensor_tensor", out=r_m2[:], in0=r_m2[:], in1=r_m1[:], op=ALU.subtract)
            S.op("act", "activation", out=r_m2[:], in_=r_m2[:], func=AF.Exp)
            V_("tensor_scalar", out=r_m1[:], in0=r_m2[:], scalar1=1.0, scalar2=None, op0=ALU.add)
            V_("reciprocal", out=r_m1[:], in_=r_m1[:])
            V_("tensor_tensor", out=r_m1[:], in0=r_m1[:], in1=r_gsum[:], op=ALU.mult)
            V_("tensor_tensor", out=r_m2[:], in0=r_m1[:], in1=r_m2[:], op=ALU.mult)
            V_("tensor_tensor", out=r_mask1[:], in0=r_mask1[:], in1=bc(r_m1[:], 16), op=ALU.mult)
            V_("tensor_tensor", out=r_mask2[:], in0=r_mask2[:], in1=bc(r_m2[:], 16), op=ALU.mult)
            V_("tensor_tensor", out=combs[:], in0=r_mask1[:], in1=r_mask2[:], op=ALU.add)

        def C0(bi, j):
            tsl = slice(bi * TB + j * 128, bi * TB + (j + 1) * 128)
            S.dma("sp", hcs[j % 2][:], st["h"][tsl, :])

        def C1(bi, j):
            macc = maccs[bi % 2]
            h_ = hcs[j % 2]
            tsl = slice(bi * TB + j * 128, bi * TB + (j + 1) * 128)
            S.op("dve", "tensor_tensor", out=macc[:, j, :], in0=macc[:, j, :], in1=GT2[:], op=ALU.mult)
            S.op("dve", "tensor_tensor", out=h_[:], in0=h_[:], in1=macc[:, j, :], op=ALU.add)
            if not final:
                S.dma("pool", st["h"][tsl, :], h_[:])

        def C2(bi, j):
            h_ = hcs[j % 2]
            S.op("act", "activation", out=junkC[:], in_=h_[:], func=AF.Square, accum_out=ssqC[:, 1:2])
            S.op("act", "activation", out=ssqC[:, 1:2], in_=ssqC[:, 1:2], func=AF.Ln, bias=RMS_EPS, scale=1.0 / D)
            S.op("act", "activation", out=ssqC[:, 1:2], in_=ssqC[:, 1:2], func=AF.Exp, scale=-0.5)

        def C3(bi, j):
            h_ = hcs[j % 2]
            tsl = slice(bi * TB + j * 128, bi * TB + (j + 1) * 128)
            S.op("dve", "scalar_tensor_tensor", out=h_[:], in0=h_[:], scalar=ssqC[:, 1:2], in1=gf[:], op0=ALU.mult, op1=ALU.mult)
            S.dma("pool", out_d[tsl, :], h_[:])

        def C_stages(bi):
            out = [[lambda: C0(bi, 0)]]
            for j in range(nsubB):
                nxt = [lambda j=j: C0(bi, j + 1)] if j + 1 < nsubB else []
                if final:
                    out.append([lambda j=j: C1(bi, j)])
                    out.append([lambda j=j: C2(bi, j)])
                    out.append([lambda j=j: C3(bi, j)] + nxt)
                else:
                    out.append([lambda j=j: C1(bi, j)] + nxt)
            return out

        ipc = [0]
        itc = [0]

        def emit_down(bi, item, idx, subs):
            e, tw0 = item
            hT = hTs[idx % 2]
            wd = wds[e % 2]
            macc, combs = maccs[bi % 2], combss[bi % 2]
            for js in subs:
                j = tw0 // 128 + js
                for half in range(2):
                    p_ = pos[ipc[0] % 2]
                    ipc[0] += 1
                    for fc in range(2):
                        S.op("pe", "matmul", _ni=True, out=p_[:], lhsT=hT[:, fc, js * 128:(js + 1) * 128], rhs=wd[:, fc, half * 512:(half + 1) * 512], start=(fc == 0), stop=(fc == 1))
                    mslice = macc[:, j, half * 512:(half + 1) * 512]
                    if e == 0:
                        S.op("dve", "tensor_scalar", out=mslice, in0=p_[:], scalar1=combs[:, j, e:e + 1], scalar2=None, op0=ALU.mult)
                    else:
                        S.op("dve", "scalar_tensor_tensor", out=mslice, in0=p_[:], scalar=combs[:, j, e:e + 1], in1=mslice, op0=ALU.mult, op1=ALU.add)

        def B_block(bi, extras):
            h2T = h2Ts[bi % 2]
            items = [(e, tw0) for e in range(NE) for tw0 in range(0, TB, TW)]
            n_it = len(items)
            slots = {}
            for k, fn in enumerate(extras):
                slots.setdefault(min(n_it - 1, (k * n_it) // max(1, len(extras))), []).append(fn)
            for idx, (e, tw0) in enumerate(items):
                wgu = wgus[e % 2]
                if tw0 == 0:
                    S.dma("sp", wgu[:], wgu_bf[e])
                    S.dma("sp", wds[e % 2][:], wd_bf[e])
                hT = hTs[idx % 2]
                for fc in range(2):
                    pg, pu, sg = pgs[itc[0] % 2], pus[itc[0] % 2], sgs[itc[0] % 2]
                    itc[0] += 1
                    for kc in range(8):
                        S.op("pe", "matmul", _ni=True, out=pg[:, 0:TW], lhsT=wgu[:, kc, fc * 128:(fc + 1) * 128], rhs=h2T[:, kc, tw0:tw0 + TW], start=(kc == 0), stop=(kc == 7))
                    for kc in range(8):
                        S.op("pe", "matmul", _ni=True, out=pu[:, 0:TW], lhsT=wgu[:, kc, FF + fc * 128:FF + (fc + 1) * 128], rhs=h2T[:, kc, tw0:tw0 + TW], start=(kc == 0), stop=(kc == 7))
                    S.op("act", "activation", out=sg[:, 0:TW], in_=pg[:, 0:TW], func=AF.Silu)
                    S.op("dve", "tensor_tensor", out=hT[:, fc, 0:TW], in0=pu[:, 0:TW], in1=sg[:, 0:TW], op=ALU.mult)
                    if idx > 0:
                        half_n = (nsubW + 1) // 2
                        subs = range(0, half_n) if fc == 0 else range(half_n, nsubW)
                        emit_down(bi, items[idx - 1], idx - 1, subs)
                for grp in slots.get(idx, []):
                    for fn in grp:
                        fn()
            emit_down(bi, items[-1], n_it - 1, range(nsubW))

        for grp in A_stages(0):
            for fn in grp:
                fn()
        for bi in range(nblk):
            extras = []
            if bi + 1 < nblk:
                extras += A_stages(bi + 1)
            if bi > 0:
                extras += C_stages(bi - 1)
            B_block(bi, extras)
        for grp in C_stages(nblk - 1):
            for fn in grp:
                fn()
        P.close()

    dbg_y = {}
    if "dbg_y" in dbg:
        dbg_y = {"lat": nc.dram_tensor("dbg_y_lat", [L, 512], F32, kind="ExternalOutput").ap(),
                 "ctx": nc.dram_tensor("dbg_y_ctx", [CTX, 512], F32, kind="ExternalOutput").ap()}

    def zero_states():
        memset("dve", Hf[:], 0.0)
        memset("dve", Hb[:], 0.0)

    class Stop(Exception):
        pass

    S.marks = []

    def mark(name):
        S.marks.append((name, dict(S.cnt)))
        if stop_after == name:
            raise Stop()

    try:
        phase_copy_in()
        mark("copy")
        for l in range(depth):
            last = l == depth - 1
            phase_ada(l)
            mark("ada%d" % l)
            phase_wprep(l)
            mark("wprep%d" % l)
            phase_p1(l, "ctx")
            mark("p1c%d" % l)
            phase_p2b(l, "ctx")
            mark("p2bc%d" % l)
            zero_states()
            if last:
                phase_state_sweep(l, "ctx", 1, False)
                phase_state_sweep(l, "ctx", 0, False)
            else:
                phase_p2(l, "ctx")
                phase_state_sweep(l, "ctx", 1, True)
                phase_p4(l, "ctx")
            mark("ctxmix%d" % l)
            phase_p1(l, "lat")
            mark("p1%d" % l)
            if multi:
                phase_exchange1()
                mark("ex1_%d" % l)
            phase_p2(l, "lat")
            mark("p2%d" % l)
            phase_p2b(l, "lat")
            mark("p2b%d" % l)
            if multi:
                S.op("dve", "tensor_copy", out=hf0[:], in_=Hf[:])
                S.op("dve", "tensor_copy", out=hb0[:], in_=Hb[:])
                zero_states()
                memset("dve", Dacc[:], 1.0)
                phase_state_sweep(l, "lat", 0, False, acc_decay=True)
                phase_state_sweep(l, "lat", 1, False)
                phase_exchange2()
                mark("ex2_%d" % l)
            phase_state_sweep(l, "lat", 1, True)
            mark("p3%d" % l)
            phase_p4(l, "lat")
            mark("p4%d" % l)
            phase_p6(l, "lat", last)
            mark("p6%d" % l)
            if not last:
                phase_p6(l, "ctx", False)
            mark("p6c%d" % l)
    except Stop:
        pass
    CP.close()
    return nc, S


def host_weights(inputs, depth=2):
    f = lambda a: np.ascontiguousarray(np.asarray(a, dtype=np.float32))
    m = {}
    for k in ("w_ada", "b_ada", "g_mix", "g_ffn", "w_in", "ssd_norm_g", "w_out", "w_gate", "w_up", "w_down", "d_skip"):
        m[k] = f(inputs[k])
    cw = np.asarray(inputs["conv_w"])
    m["conv_wT"] = f(cw.transpose(0, 2, 1).reshape(depth, 4, 128, 31).transpose(0, 2, 1, 3))
    pc = np.stack([np.asarray(inputs["conv_b"]), np.asarray(inputs["conv_ln_g"]), np.asarray(inputs["conv_ln_b"])], axis=1)
    m["conv_pc"] = f(pc.reshape(depth, 3, 4, 128).transpose(0, 3, 1, 2))
    sw = np.asarray(inputs["ssd_conv_w"])
    m["sconv_wT"] = f(sw.transpose(0, 2, 1).reshape(depth, 8, 128, 5).transpose(0, 2, 1, 3))
    m["sconv_b"] = f(np.asarray(inputs["ssd_conv_b"]).reshape(depth, 8, 128).transpose(0, 2, 1))
    m["dt_bias"] = f(np.asarray(inputs["dt_bias"]).reshape(depth, 16))
    m["a_log"] = f(np.asarray(inputs["a_log"]).reshape(depth, 16))
    m["w_r"] = f(np.concatenate([inputs["w_router_group"], inputs["w_router_expert"]], axis=2))
    m["b_r"] = f(np.concatenate([inputs["b_router_group"], inputs["b_router_expert"]], axis=1))
    m["g_final"] = f(np.asarray(inputs["g_final"]).reshape(1, D))
    return m


def host_core_map(inputs, wmap, k, ncores, nq):
    f = lambda a: np.ascontiguousarray(np.asarray(a, dtype=np.float32))
    b, q = k // nq, k % nq
    Ltot = np.asarray(inputs["x"]).shape[1]
    LQ = Ltot // nq
    m = dict(wmap)
    m["x"] = f(inputs["x"][b, q * LQ:(q + 1) * LQ])
    m["ctx"] = f(inputs["ctx"][b])
    m["cvT"] = f(np.stack([inputs["c"][b], inputs["c_ctx"]], axis=1))
    msk = np.zeros((1, 32), np.float32)
    for j in range(ncores):
        same = (j // nq) == b
        if same and j == k - 1:
            msk[0, j] = 1.0
        if same and j == k + 1:
            msk[0, 8 + j] = 1.0
        if same and j < k:
            msk[0, 16 + j] = 1.0
        if same and j > k:
            msk[0, 24 + j] = 1.0
    m["msk"] = msk
    return m


_CACHE = {}
NQ = 1
NCORES = 2


def kernel(**inputs):
    x = np.asarray(inputs["x"])
    B, L, _ = x.shape
    CTX = np.asarray(inputs["ctx"]).shape[1]
    depth = np.asarray(inputs["w_in"]).shape[0]
    nwork = B * NQ
    LQ = L // NQ
    key = (LQ, CTX, depth, nwork if NQ > 1 else 1)
    if key not in _CACHE:
        _CACHE[key] = build(LQ, CTX, depth, ncores=key[3])[0]
    nc = _CACHE[key]
    wmap = host_weights(inputs, depth)
    work = [host_core_map(inputs, wmap, k, nwork, NQ) for k in range(nwork)]
    in_maps = [work[k % nwork] for k in range(NCORES)]
    res = run_bass_kernel_spmd(nc, in_maps, core_ids=list(range(NCORES)))
    out = np.empty((B, L, D), np.float32)
    for k in range(nwork):
        b, q = k // NQ, k % NQ
        out[b, q * LQ:(q + 1) * LQ] = np.asarray(res.results[k]["out"])
    return out
```
